# Optimizing a Trainium2 kernel written in Bass

```python
import math
import jax
import jax.numpy as jnp
from jax import lax
import numpy as np

D_MODEL = 1024
BATCH = 2
SEQ = 8192
DEPTH = 2

CTX_LEN = 256
GRID_W = 64
N_MIXERS = 2
N_HGRN_LAYERS = (DEPTH + N_MIXERS - 1) // N_MIXERS
N_DIFF_LAYERS = DEPTH // N_MIXERS
HGRN_HEADS = 8
HGRN_EXPAND = 128
HGRN_FDIM = HGRN_HEADS * HGRN_EXPAND
HGRN_HEAD_V = D_MODEL // HGRN_HEADS
CHUNK = 64
DIFF_HEADS = 8
DIFF_HEAD_DIM = D_MODEL // DIFF_HEADS // 2
ROPE_BASE = 10000.0
ROPE_PAIRS_PER_AXIS = DIFF_HEAD_DIM // 4
Q_BLOCK = 128
N_EXPERTS = 16
EC_CAPACITY_FACTOR = 2
D_EXPERT = 2816
NORM_EPS = 1e-6

kernel_name = "hybrid_hgrn2_diffattn_ecmoe_dit"


def _rmsnorm(x, g):
    xf = x.astype(jnp.float32)
    y = xf * lax.rsqrt(jnp.mean(xf * xf, axis=-1, keepdims=True) + NORM_EPS)
    return (y * g.astype(jnp.float32)).astype(x.dtype)


def _gla_step(S, inp):
    q, k, v, lf = inp
    b = jnp.cumsum(lf, axis=2)
    tril = jnp.tril(jnp.ones((CHUNK, CHUNK), dtype=bool))
    o_inter = jnp.einsum('bhtk,bhkv->bhtv', q * jnp.exp(b), S)
    rel = b[:, :, :, None, :] - b[:, :, None, :, :]
    decay = jnp.exp(jnp.where(tril[:, :, None], rel, -jnp.inf))
    a = jnp.einsum('bhtk,bhsk,bhtsk->bhts', q, k, decay)
    o = o_inter + jnp.einsum('bhts,bhsv->bhtv', a, v)
    b_last = b[:, :, -1:, :]
    S_new = jnp.exp(b_last[:, :, 0, :])[..., None] * S + jnp.einsum(
        'bhsk,bhsv->bhkv', k * jnp.exp(b_last - b), v)
    return S_new, o


def _gla_chunked(q, k, v, lf, S0):
    B, T, H, _ = q.shape
    V = v.shape[-1]
    n = T // CHUNK

    def to_blocks(t):
        return t.reshape(B, n, CHUNK, H, t.shape[-1]).transpose(1, 0, 3, 2, 4)

    S, o = lax.scan(_gla_step, S0, (to_blocks(q), to_blocks(k), to_blocks(v), to_blocks(lf)))
    return S, o.transpose(1, 0, 3, 2, 4).reshape(B, T, H, V)


def _hgrn2_mixer(h_ctx, h_lat, w_in, lb, norm_g, w_out):
    splits = [HGRN_FDIM, 2 * HGRN_FDIM, 3 * HGRN_FDIM, 3 * HGRN_FDIM + D_MODEL]

    def project(h):
        B, T, _ = h.shape
        q, f_fw, f_bw, v, g = jnp.split(h @ w_in, splits, axis=-1)
        hd = lambda t, d: t.reshape(B, T, HGRN_HEADS, d).astype(jnp.float32)
        return (jax.nn.silu(hd(q, HGRN_EXPAND)), hd(f_fw, HGRN_EXPAND), hd(f_bw, HGRN_EXPAND),
                hd(v, HGRN_HEAD_V), g)

    def gates(f_logit):
        f = lb + (1.0 - lb) * jax.nn.sigmoid(f_logit)
        return jnp.log(f), 1.0 - f

    qc, fc_fw, fc_bw, vc, gc = project(h_ctx)
    ql, fl_fw, fl_bw, vl, gl = project(h_lat)
    B = h_lat.shape[0]

    def direction(fc, fl, reverse):
        lfc, kc = gates(fc)
        lfl, kl = gates(fl)
        flip = (lambda t: jnp.flip(t, axis=1)) if reverse else (lambda t: t)
        S0 = jnp.zeros((B, HGRN_HEADS, HGRN_EXPAND, HGRN_HEAD_V), jnp.float32)
        S_ctx, oc = _gla_chunked(flip(qc), flip(kc), flip(vc), flip(lfc), S0)
        _, ol = _gla_chunked(flip(ql), flip(kl), flip(vl), flip(lfl), S_ctx)
        return flip(oc), flip(ol)

    oc_f, ol_f = direction(fc_fw, fl_fw, False)
    oc_b, ol_b = direction(fc_bw, fl_bw, True)

    def readout(o, g):
        Bo, T = o.shape[:2]
        o = _rmsnorm(o, norm_g) * jax.nn.silu(g.astype(jnp.float32)).reshape(
            Bo, T, HGRN_HEADS, HGRN_HEAD_V)
        return o.reshape(Bo, T, D_MODEL).astype(g.dtype) @ w_out

    return readout(oc_f + oc_b, gc), readout(ol_f + ol_b, gl)


def _axial_rope_tables(T):
    rows = T // GRID_W
    row = jnp.repeat(jnp.arange(rows, dtype=jnp.float32), GRID_W)
    col = jnp.tile(jnp.arange(GRID_W, dtype=jnp.float32), rows)
    freq = ROPE_BASE ** (-jnp.arange(ROPE_PAIRS_PER_AXIS, dtype=jnp.float32) / ROPE_PAIRS_PER_AXIS)
    ang = jnp.concatenate([row[:, None] * freq, col[:, None] * freq], axis=-1)
    return jnp.cos(ang), jnp.sin(ang)


def _apply_rope(x, cos, sin):
    c = cos[None, :, None, None, :]
    s = sin[None, :, None, None, :]
    x1, x2 = jnp.split(x, 2, axis=-1)
    return jnp.concatenate([x1 * c - x2 * s, x2 * c + x1 * s], axis=-1)


def _diff_scores(qb, keys, vals, lam):
    s = jnp.einsum('bqhcd,bkhcd->bhcqk', qb, keys) * (DIFF_HEAD_DIM ** -0.5)
    p = jax.nn.softmax(s, axis=-1)
    w = p[:, :, 0] - lam * p[:, :, 1]
    return jnp.einsum('bhqk,bkhv->bqhv', w, vals)


def _diff_attention(h_ctx, h_lat, w_in, lam_vecs, subln_g, w_out, lam_init, need_ctx):
    def project(h):
        B, T, _ = h.shape
        q, k, v = jnp.split(h @ w_in, 3, axis=-1)
        q = q.reshape(B, T, DIFF_HEADS, 2, DIFF_HEAD_DIM).astype(jnp.float32)
        k = k.reshape(B, T, DIFF_HEADS, 2, DIFF_HEAD_DIM).astype(jnp.float32)
        v = v.reshape(B, T, DIFF_HEADS, 2 * DIFF_HEAD_DIM).astype(jnp.float32)
        return q, k, v

    qc, kc, vc = project(h_ctx)
    ql, kl, vl = project(h_lat)
    B, T = h_lat.shape[:2]
    cos, sin = _axial_rope_tables(T)
    ql = _apply_rope(ql, cos, sin)
    kl = _apply_rope(kl, cos, sin)
    lv = lam_vecs.astype(jnp.float32)
    lam = jnp.exp(jnp.sum(lv[0] * lv[1])) - jnp.exp(jnp.sum(lv[2] * lv[3])) + lam_init

    keys = jnp.concatenate([kc, kl], axis=1)
    vals = jnp.concatenate([vc, vl], axis=1)
    nb = T // Q_BLOCK
    qblk = ql.reshape(B, nb, Q_BLOCK, DIFF_HEADS, 2, DIFF_HEAD_DIM).transpose(1, 0, 2, 3, 4, 5)
    ol = lax.map(lambda qb: _diff_scores(qb, keys, vals, lam), qblk)
    ol = ol.transpose(1, 0, 2, 3, 4).reshape(B, T, DIFF_HEADS, 2 * DIFF_HEAD_DIM)

    def readout(o):
        Bo, To = o.shape[:2]
        o = _rmsnorm(o, subln_g) * (1.0 - lam_init)
        return o.reshape(Bo, To, D_MODEL).astype(h_lat.dtype) @ w_out

    y_lat = readout(ol)
    y_ctx = readout(_diff_scores(qc, kc, vc, lam)) if need_ctx else None
    return y_ctx, y_lat


def _ec_moe(h, w_router, w_gate, w_up, w_down):
    B, T, _ = h.shape
    cap = EC_CAPACITY_FACTOR * T // N_EXPERTS
    aff = jax.nn.softmax((h @ w_router).astype(jnp.float32), axis=-1)
    g, idx = lax.top_k(jnp.swapaxes(aff, 1, 2), cap)
    xs = jax.vmap(lambda hb, ib: hb[ib])(h, idx)
    a = jnp.einsum('becd,edf->becf', xs, w_gate)
    u = jnp.einsum('becd,edf->becf', xs, w_up)
    y = jnp.einsum('becf,efd->becd', jax.nn.silu(a) * u, w_down) * g[..., None].astype(h.dtype)
    bidx = jnp.arange(B)[:, None, None]
    return jnp.zeros_like(h).at[bidx, idx].add(y.astype(h.dtype))


def setup_inputs(seed: int = 0) -> dict:
    key = jax.random.key(seed)
    ks = jax.random.split(key, 24)
    nrm = lambda k, shape, s: jax.random.normal(k, shape, jnp.float32) * s
    D = D_MODEL
    return {
        "x": nrm(ks[0], (BATCH, SEQ, D), 1.0),
        "c": nrm(ks[1], (BATCH, D), 1.0),
        "ctx": nrm(ks[2], (BATCH, CTX_LEN, D), 1.0),
        "c_ctx": nrm(ks[3], (D,), 1.0),
        "ada_w": nrm(ks[4], (DEPTH, D, 6 * D), 0.5 * D ** -0.5),
        "ada_b": nrm(ks[5], (DEPTH, 6 * D), 0.02),
        "norm_mix": 1.0 + nrm(ks[6], (DEPTH, D), 0.05),
        "norm_ffn": 1.0 + nrm(ks[7], (DEPTH, D), 0.05),
        "norm_final": 1.0 + nrm(ks[8], (D,), 0.05),
        "hgrn_w_in": nrm(ks[9], (N_HGRN_LAYERS, D, 3 * HGRN_FDIM + 2 * D), D ** -0.5),
        "hgrn_lb_logits": nrm(ks[10], (N_HGRN_LAYERS + 1, HGRN_FDIM), 0.5),
        "hgrn_norm": 1.0 + nrm(ks[11], (N_HGRN_LAYERS, HGRN_HEAD_V), 0.05),
        "hgrn_w_out": nrm(ks[12], (N_HGRN_LAYERS, D, D), D ** -0.5),
        "diff_w_in": nrm(ks[13], (N_DIFF_LAYERS, D, 3 * D), D ** -0.5),
        "diff_lambda": nrm(ks[14], (N_DIFF_LAYERS, 4, DIFF_HEAD_DIM), 0.1),
        "diff_subln": 1.0 + nrm(ks[15], (N_DIFF_LAYERS, 2 * DIFF_HEAD_DIM), 0.05),
        "diff_w_out": nrm(ks[16], (N_DIFF_LAYERS, D, D), D ** -0.5),
        "moe_router": nrm(ks[17], (DEPTH, D, N_EXPERTS), D ** -0.5),
        "moe_w_gate": nrm(ks[18], (DEPTH, N_EXPERTS, D, D_EXPERT), D ** -0.5),
        "moe_w_up": nrm(ks[19], (DEPTH, N_EXPERTS, D, D_EXPERT), D ** -0.5),
        "moe_w_down": nrm(ks[20], (DEPTH, N_EXPERTS, D_EXPERT, D), D_EXPERT ** -0.5),
    }


def reference(x, c, ctx, c_ctx, ada_w, ada_b, norm_mix, norm_ffn, norm_final,
              hgrn_w_in, hgrn_lb_logits, hgrn_norm, hgrn_w_out,
              diff_w_in, diff_lambda, diff_subln, diff_w_out,
              moe_router, moe_w_gate, moe_w_up, moe_w_down):
    lower_bounds = jnp.cumsum(jax.nn.softmax(hgrn_lb_logits.astype(jnp.float32), axis=0), axis=0)
    silu_c = jax.nn.silu(c)
    silu_cc = jax.nn.silu(c_ctx)
    x_lat, x_ctx = x, ctx
    for i in range(DEPTH):
        last = i == DEPTH - 1
        j = i // N_MIXERS
        mod_l = (silu_c @ ada_w[i] + ada_b[i])[:, None, :]
        mod_c = (silu_cc @ ada_w[i] + ada_b[i])[None, None, :]
        sh_m, sc_m, gt_m, sh_f, sc_f, gt_f = jnp.split(mod_l, 6, axis=-1)
        csh_m, csc_m, cgt_m, csh_f, csc_f, cgt_f = jnp.split(mod_c, 6, axis=-1)

        h_lat = _rmsnorm(x_lat, norm_mix[i]) * (1.0 + sc_m) + sh_m
        h_ctx = _rmsnorm(x_ctx, norm_mix[i]) * (1.0 + csc_m) + csh_m
        if i % N_MIXERS == 0:
            lb = lower_bounds[j].reshape(HGRN_HEADS, HGRN_EXPAND)
            y_ctx, y_lat = _hgrn2_mixer(h_ctx, h_lat, hgrn_w_in[j], lb, hgrn_norm[j], hgrn_w_out[j])
        else:
            lam_init = 0.8 - 0.6 * math.exp(-0.3 * i)
            y_ctx, y_lat = _diff_attention(h_ctx, h_lat, diff_w_in[j], diff_lambda[j], diff_subln[j],
                                           diff_w_out[j], lam_init, not last)
        x_lat = x_lat + gt_m * y_lat
        h_lat = _rmsnorm(x_lat, norm_ffn[i]) * (1.0 + sc_f) + sh_f
        x_lat = x_lat + gt_f * _ec_moe(h_lat, moe_router[i], moe_w_gate[i], moe_w_up[i], moe_w_down[i])
        if not last:
            x_ctx = x_ctx + cgt_m * y_ctx
            h_ctx = _rmsnorm(x_ctx, norm_ffn[i]) * (1.0 + csc_f) + csh_f
            x_ctx = x_ctx + cgt_f * _ec_moe(h_ctx, moe_router[i], moe_w_gate[i], moe_w_up[i], moe_w_down[i])
    return _rmsnorm(x_lat, norm_final)
```

```python
from contextlib import ExitStack
import math
import numpy as np
import ml_dtypes
import concourse.bass as bass
import concourse.mybir as mybir
from concourse.bass_utils import run_bass_kernel_spmd

F32 = mybir.dt.float32
BF16 = mybir.dt.bfloat16
I32 = mybir.dt.int32
AF = mybir.ActivationFunctionType
ALU = mybir.AluOpType
AX = mybir.AxisListType

NCORES = 8
D = 1024
KD = 8
NL = 2048
NCX = 256
NT = NL + NCX
EPS = 1e-6
NE = 16
DEXP = 2816
NF = DEXP // 128
CAP_L = 1024
CAP_C = 32
DIFF_SCALE = 0.125

COMPUTE = ("pe", "act", "dve", "pool")
QUEUES = ("sp",)


class Sched:
    def __init__(self, nc):
        self.nc = nc
        self.streams = {e: [] for e in COMPUTE + QUEUES}
        self.cnt = {}
        self.seen = {e: {} for e in COMPUTE + QUEUES}
        self.last_w = {}
        self.readers = {}
        self.n_ins = 0
        self.n_wait = 0

    def _deps(self, reads, writes):
        deps = {}

        def add(sk, n):
            if deps.get(sk, 0) < n:
                deps[sk] = n

        for r in reads:
            lw = self.last_w.get(r)
            if lw:
                add(*lw)
        for w in writes:
            lw = self.last_w.get(w)
            if lw:
                add(*lw)
            for sk, n in self.readers.get(w, {}).items():
                add(sk, n)
        return deps

    def _emit_waits(self, eng, deps, skip_self=False):
        for sk, n in deps.items():
            if skip_self and sk == eng:
                continue
            if self.seen[eng].get(sk, 0) >= n:
                continue
            self.seen[eng][sk] = n
            self.streams[eng].append(("wait", sk, n))
            self.n_wait += 1

    def _record(self, sk, n, reads, writes):
        for r in reads:
            d = self.readers.setdefault(r, {})
            if d.get(sk, 0) < n:
                d[sk] = n
        for w in writes:
            self.last_w[w] = (sk, n)
            self.readers[w] = {}

    def op(self, eng, fn, reads=(), writes=()):
        deps = self._deps(reads, writes)
        self._emit_waits(eng, deps, skip_self=(eng == "pe"))
        n = self.cnt.get(eng, 0) + 1
        self.cnt[eng] = n
        self.streams[eng].append(("ins", fn, eng, 1))
        self._record(eng, n, reads, writes)
        self.n_ins += 1

    def dma(self, out, in_, reads=(), writes=(), slot=None, q="sp", **kw):
        if slot is None:
            slot = writes[0]
        sk = ("dma", slot)
        deps = self._deps(reads, writes)
        self._emit_waits(q, deps)
        n = self.cnt.get(sk, 0) + 1
        self.cnt[sk] = n
        self.streams[q].append(("ins", lambda e: e.dma_start(out=out, in_=in_, **kw), sk, 16))
        self._record(sk, n, reads, writes)
        self.n_ins += 1

    def coll(self, fn, reads=(), writes=()):
        self.op("pool", fn, reads=reads, writes=writes)

    def wait_all(self, eng, bufkeys):
        self._emit_waits(eng, self._deps(bufkeys, ()))

    def barrier(self):
        snap = dict(self.cnt)
        for eng in COMPUTE + QUEUES:
            self._emit_waits(eng, snap)

    def emit(self):
        nc = self.nc
        semkeys = list(self.cnt.keys())
        assert len(semkeys) <= 100, len(semkeys)
        with ExitStack() as st:
            sems = {}
            for i, sk in enumerate(semkeys):
                sems[sk] = st.enter_context(nc.semaphore("s%d" % i))
            block = st.enter_context(nc.Block())

            def run(eng_name):
                def body(e):
                    for item in self.streams[eng_name]:
                        if item[0] == "wait":
                            _, sk, n = item
                            mult = 16 if isinstance(sk, tuple) else 1
                            e.wait_ge(sems[sk], n * mult)
                        else:
                            _, fn, sk, inc = item
                            fn(e).then_inc(sems[sk], inc)
                return body

            block.tensor(run("pe"))
            block.scalar(run("act"))
            block.vector(run("dve"))
            block.gpsimd(run("pool"))
            block.sync(run("sp"))


class Ring:
    def __init__(self, aps, name):
        self.aps = aps
        self.name = name
        self.i = 0

    def next(self):
        j = self.i % len(self.aps)
        self.i += 1
        return self.aps[j], "%s%d" % (self.name, j)


class Builder:
    SB_BASE = 16512
    SB_TOP = 229344

    def __init__(self, stage, dbg):
        self.stage = stage
        self.dbg = dbg
        self.nc = bass.Bass("TRN2", target_bir_lowering=False)
        self.S = Sched(self.nc)
        self.off = self.SB_BASE
        self.names = 0
        self.ps_banks = []

    def sb(self, shape, dt, name=None):
        esz = 4 if dt in (F32, I32) else 2
        sz = int(np.prod(shape[1:])) * esz
        sz = (sz + 31) // 32 * 32
        self.names += 1
        nm = "%s_%d" % (name or "t", self.names)
        t = self.nc.alloc_sbuf_tensor_at(nm, list(shape), dt, offset=self.off)
        self.off += sz
        assert self.off <= self.SB_TOP, ("SBUF overflow", nm, self.off)
        return t

    def mark(self):
        return self.off

    def release(self, mark):
        self.S.barrier()
        self.off = mark

    def ring(self, n, shape, dt, name):
        return Ring([self.sb(shape, dt, name) for _ in range(n)], name + "_%d_" % self.names)

    def dram_in(self, name, shape, dt=F32):
        return self.nc.dram_tensor(name, list(shape), dt, kind="ExternalInput").ap()

    def dram_out(self, name, shape, dt=F32):
        return self.nc.dram_tensor(name, list(shape), dt, kind="ExternalOutput").ap()

    def dram_tmp(self, name, shape, dt=F32):
        return self.nc.dram_tensor(name, list(shape), dt).ap()

    def mm(self, out, lhsT, rhs, start, stop, r, w):
        self.S.op("pe", lambda e: e.matmul(out, lhsT=lhsT, rhs=rhs, start=start, stop=stop), reads=r, writes=w)

    def tr(self, out, in_, ident, r, w):
        self.S.op("pe", lambda e: e.transpose(out, in_, ident), reads=r, writes=w)

    def act(self, out, in_, func, r, w, scale=1.0, bias=0.0):
        self.S.op("act", lambda e: e.activation(out=out, in_=in_, func=func, bias=bias, scale=scale), reads=r, writes=w)

    def tt(self, eng, out, a, b_, op, r, w):
        self.S.op(eng, lambda e: e.tensor_tensor(out=out, in0=a, in1=b_, op=op), reads=r, writes=w)

    def ts(self, eng, out, a, s1, s2, op0, op1, r, w):
        if s2 is None:
            self.S.op(eng, lambda e: e.tensor_scalar(out=out, in0=a, scalar1=s1, scalar2=None, op0=op0), reads=r, writes=w)
        else:
            self.S.op(eng, lambda e: e.tensor_scalar(out=out, in0=a, scalar1=s1, scalar2=s2, op0=op0, op1=op1), reads=r, writes=w)

    def stt(self, eng, out, in0, scalar, in1, op0, op1, r, w):
        self.S.op(eng, lambda e: e.scalar_tensor_tensor(out=out, in0=in0, scalar=scalar, in1=in1, op0=op0, op1=op1), reads=r, writes=w)

    def cp(self, eng, out, in_, r, w):
        if eng == "act":
            self.S.op("act", lambda e: e.copy(out=out, in_=in_), reads=r, writes=w)
        else:
            self.S.op(eng, lambda e: e.tensor_copy(out=out, in_=in_), reads=r, writes=w)

    def rsqrt(self, out, in_, r, wk, eps=EPS):
        self.act(out, in_, AF.Sqrt, list(r) + ["epsT"], [wk], bias=self.eps_ap(eps))
        self.S.op("dve", lambda e: e.reciprocal(out=out, in_=out), reads=[wk], writes=[wk])

    def eps_ap(self, eps):
        return self.epsT[:, 0:1]

    def memset(self, eng, ap, val, w):
        self.S.op(eng, lambda e: e.memset(ap, val), reads=(), writes=w)


def col_tiles(with_ctx=True):
    t = [(i * 512, 512, False) for i in range(NL // 512)]
    if with_ctx:
        t.append((NL, NCX, True))
    return t


def build(stage=99, dbg=False):
    B = Builder(stage, dbg)
    nc, S = B.nc, B.S

    xT_d = B.dram_in("xT", [D, NT])
    cT_d = B.dram_in("cT", [D, 2])
    ada_w_d = B.dram_in("ada_w", [2, D, 6 * D])
    adab_d = B.dram_in("adab", [128, 2, 48])
    vecs_d = B.dram_in("vecs", [128, 64])
    cmat_d = B.dram_in("cmat", [128, 4, 128])
    masks_d = B.dram_in("masks", [128, 2, 128], I32)
    scanm_d = B.dram_in("scanm", [128, 512])
    segm_d = B.dram_in("segm", [128, 16])
    hw_in_d = B.dram_in("hgrn_w_in", [D, 5 * D])
    hw_out_d = B.dram_in("hgrn_w_out", [D, D])
    dw_in_d = B.dram_in("diff_w_in", [D, 3 * D])
    dw_out_d = B.dram_in("diff_w_out", [D, D])
    lvb_d = B.dram_in("lvb", [128, 256])
    rope_d = B.dram_in("rope", [128, 2, NL])
    cm2_d = B.dram_in("cm2", [128, 18, 128])
    router_d = B.dram_in("moe_router", [2, D, NE])
    wgate_d = B.dram_in("moe_w_gate", [2, NE, D, DEXP])
    wup_d = B.dram_in("moe_w_up", [2, NE, D, DEXP])
    wdown_d = B.dram_in("moe_w_down", [2, NE, DEXP, D])
    out_d = B.dram_out("outT", [D, NL])
    dbg_d = B.dram_out("dbg", [D, NT]) if dbg else None

    x_sb = B.sb([128, KD, NT], F32, "x")
    h_sb = B.sb([128, KD, NT], BF16, "h")
    cmat = B.sb([128, 4, 128], F32, "cmat")
    ident_f = cmat[:, 0, :]
    ident_b = B.sb([128, 128], BF16, "identb")
    ones_b = B.sb([128, 128], BF16, "onesb")
    mean_b = B.sb([128, 128], BF16, "meanb")
    m128_b = B.sb([128, 128], BF16, "m128b")
    masks = B.sb([128, 2, 128], I32, "masks")
    scanm = B.sb([128, 512], F32, "scanm")
    segm = B.sb([128, 16], F32, "segm")
    vecs = B.sb([128, 64], F32, "vecs")
    modT = B.sb([128, 2, 48, 2], F32, "modT")
    adab = B.sb([128, 2, 48], F32, "adab")
    coefA = B.sb([128, 4, 2, KD], F32, "coefA")
    siluc = B.sb([128, KD, 2], F32, "siluc")
    lb = B.sb([128, 8], F32, "lb")
    B.epsT = B.sb([128, 2], F32, "epsT")
    B.memset("pool", B.epsT[:], EPS, ["epsT"])
    oml = B.sb([128, 8], F32, "oml")

    PS = [nc.alloc_psum_tensor("ps%d" % i, [128, 512], F32) for i in range(8)]

    for k in range(KD):
        S.dma(x_sb[:, k, :], xT_d[k * 128:(k + 1) * 128, :], writes=["x%d" % k])
    S.dma(cmat[:], cmat_d, writes=["cmat"])
    S.dma(masks[:], masks_d, writes=["masks"])
    S.dma(scanm[:], scanm_d, writes=["scanm"])
    S.dma(segm[:], segm_d, writes=["segm"])
    S.dma(vecs[:], vecs_d, writes=["vecs"])
    S.dma(adab[:], adab_d, writes=["adab"])
    S.dma(siluc[:], cT_d.rearrange("(k p) c -> p k c", p=128), writes=["siluc"])
    B.cp("dve", ident_b[:], cmat[:, 0, :], ["cmat"], ["identb"])
    B.cp("dve", ones_b[:], cmat[:, 1, :], ["cmat"], ["onesb"])
    B.ts("dve", mean_b[:], cmat[:, 1, :], 1.0 / D, None, ALU.mult, None, ["cmat"], ["meanb"])
    B.ts("dve", m128_b[:], cmat[:, 1, :], 1.0 / 128, None, ALU.mult, None, ["cmat"], ["m128b"])
    B.act(siluc[:], siluc[:], AF.Silu, ["siluc"], ["siluc"])
    B.tt("dve", lb[:], vecs[:, 40:48], vecs[:, 48:56], ALU.subtract, ["vecs"], ["lb"])
    B.act(lb[:], lb[:], AF.Sigmoid, ["lb"], ["lb"])
    B.ts("dve", oml[:], lb[:], -1.0, 1.0, ALU.mult, ALU.add, ["lb"], ["oml"])

    m0 = B.mark()
    adaring = B.ring(2, [128, KD, 768], F32, "adaw")
    for i in range(2):
        for blk in range(8):
            wt, wk = adaring.next()
            S.dma(wt[:], ada_w_d[i, :, blk * 768:(blk + 1) * 768].rearrange("(k p) j -> p k j", p=128), writes=[wk])
            for jj in range(6):
                j = blk * 6 + jj
                for k in range(KD):
                    B.mm(PS[0][:, j * 2:(j + 1) * 2], wt[:, k, jj * 128:(jj + 1) * 128], siluc[:, k, :],
                         k == 0, k == KD - 1, [wk, "siluc"], ["ps0"])
        for col in range(2):
            B.tt("dve", modT[:, i, :, col], PS[0][:, 0:96].rearrange("p (j c) -> p j c", c=2)[:, :, col],
                 adab[:, i, :], ALU.add, ["ps0", "adab"], ["modT"])
    for n in range(4):
        layer, ffn = n // 2, n % 2
        gcol = (16 if ffn else 0) + 8 * layer
        sc0 = 32 if ffn else 8
        for col in range(2):
            B.stt("dve", coefA[:, n, col, :], modT[:, layer, sc0:sc0 + 8, col], 1.0, vecs[:, gcol:gcol + 8],
                  ALU.add, ALU.mult, ["modT", "vecs"], ["coefA"])
    B.release(m0)

    def norm_mod(n, with_ctx=True):
        layer, ffn = n // 2, n % 2
        sh0 = 24 if ffn else 0
        mk = B.mark()
        sqr = B.ring(3, [128, 512], BF16, "sq")
        rsr = B.ring(2, [128, 512], F32, "rstd")
        tmr = B.ring(3, [128, 512], F32, "ntmp")
        for (c0, cw, isctx) in col_tiles(with_ctx):
            col = 1 if isctx else 0
            for k in range(KD):
                sq, sqk = sqr.next()
                B.act(sq[:, :cw], x_sb[:, k, c0:c0 + cw], AF.Square, ["x%d" % k], [sqk])
                B.mm(PS[1][:, :cw], mean_b[:], sq[:, :cw], k == 0, k == KD - 1, [sqk, "meanb"], ["ps1"])
            rs, rsk = rsr.next()
            B.rsqrt(rs[:, :cw], PS[1][:, :cw], ["ps1"], rsk)
            for k in range(KD):
                tm, tmk = tmr.next()
                B.tt("dve", tm[:, :cw], x_sb[:, k, c0:c0 + cw], rs[:, :cw], ALU.mult, ["x%d" % k, rsk], [tmk])
                B.act(h_sb[:, k, c0:c0 + cw], tm[:, :cw], AF.Identity, [tmk, "coefA", "modT"], ["h%d" % k],
                      scale=coefA[:, n, col, k:k + 1], bias=modT[:, layer, sh0 + k, col:col + 1])
        B.release(mk)

    norm_mod(0)

    def finish(dump=None):
        if dbg and dump is not None:
            if dump == "x":
                for k in range(KD):
                    S.dma(dbg_d[k * 128:(k + 1) * 128, :], x_sb[:, k, :], reads=["x%d" % k], writes=["dbg%d" % k])
            else:
                mk = B.mark()
                hf = B.sb([128, NT], F32, "hf")
                for k in range(KD):
                    B.cp("dve", hf[:], h_sb[:, k, :], ["h%d" % k], ["hf"])
                    S.dma(dbg_d[k * 128:(k + 1) * 128, :], hf[:], reads=["hf"], writes=["dbg%d" % k])
            S.wait_all("sp", ["dbg%d" % k for k in range(KD)])
        for k in range(KD):
            S.dma(out_d[k * 128:(k + 1) * 128, :], x_sb[:, k, 0:NL], reads=["x%d" % k], writes=["out%d" % k])
        S.wait_all("sp", ["out%d" % k for k in range(KD)])
        S.emit()
        return nc

    if stage == 1:
        return finish("h")

    noml = B.sb([128, 8], F32, "noml")
    B.ts("dve", noml[:], oml[:], -1.0, None, ALU.mult, None, ["oml"], ["noml"])
    cin_h = [B.dram_tmp("cin_h%d" % i, [512, 256]) for i in range(4)]
    cout_h = [B.dram_tmp("cout_h%d" % i, [2048, 256]) for i in range(4)]
    PSB = nc.alloc_psum_tensor("psb", [128, 1024], BF16) if False else None
    PS4b = PS[4][:].bitcast(BF16)

    mh = B.mark()
    wst = B.ring(2, [128, KD, 128], F32, "wst")
    wbf = B.ring(6, [128, KD, 128], BF16, "wbf")
    q_sb = B.sb([128, NT], BF16, "q")
    g_sb = B.sb([128, NT], BF16, "g")
    v_tok = B.sb([128, NT // 128, 128], BF16, "vtok")
    o_acc = B.sb([128, NT], F32, "oacc")
    tmp = B.ring(6, [128, 512], F32, "tmp")
    qbr = B.ring(2, [128, 512], BF16, "qb")
    kbr = B.ring(2, [128, 512], BF16, "kb")
    kdr = B.ring(2, [128, 512], BF16, "kd")
    kdTr = B.ring(2, [128, 2, 4, 128], BF16, "kdT")
    for t_ in kdTr.aps:
        _, k_ = kdTr.next()
        B.memset("pool", t_[:], 0.0, [k_])
    sqr_ = B.ring(2, [128, 8, 128], BF16, "Sq")
    usbr = B.ring(2, [128, 2, 4, 128], F32, "Usb")
    aTf = B.ring(4, [128, 128], BF16, "aTf")
    aTb = B.ring(4, [128, 128], BF16, "aTb")
    for r_ in (aTf, aTb):
        for t_ in r_.aps:
            _, k_ = r_.next()
            B.memset("pool", t_[:], 0.0, [k_])
    srun = B.ring(3, [128, 128], F32, "srun")
    small = B.ring(8, [128, 8], F32, "small")
    lbuf = B.ring(1, [128, 4, 129], F32, "lbuf")
    onr = B.ring(2, [128, 512], BF16, "on")
    wo_st = B.sb([128, D], F32, "wost")
    wo_bf = B.sb([128, D], BF16, "wobf")
    totacc = B.sb([128, 8], F32, "totacc")

    def load_w(j):
        st_, stk = wst.next()
        S.dma(st_[:], hw_in_d[:, j * 128:(j + 1) * 128].rearrange("(k p) c -> p k c", p=128), writes=[stk])
        wb, wbk = wbf.next()
        B.cp("pool", wb[:], st_[:], [stk], [wbk])
        return wb, wbk

    def proj_fm(ps, psk, wb, wbk, c0, cw):
        for k in range(KD):
            B.mm(ps[:, :cw], wb[:, k, :], h_sb[:, k, c0:c0 + cw], k == 0, k == KD - 1, [wbk, "h%d" % k], [psk])

    import os
    HSTOP = int(os.environ.get("HSTOP", "99"))
    HFULL = int(os.environ.get("HFULL", "99"))

    def hgrn_dir(hd, d_, full, wf, wfk):
        lat_tiles = [(i * 512, 512, False) for i in range(4)]
        if d_ == 1:
            lat_tiles = lat_tiles[::-1]
        tiles = ([(NL, NCX, True)] if full else []) + lat_tiles
        S_cur, S_k = srun.next()
        B.memset("pool", S_cur[:], 0.0, [S_k])
        aTr = aTf if d_ == 0 else aTb
        first_lat = True
        ti = 0
        for (c0, cw, isctx) in tiles:
            nch = cw // 64
            nsub = cw // 128
            if full and (not isctx) and first_lat:
                lb_, lbk = lbuf.next()
                for s_ in range(4):
                    p_ = hd * 2 + d_
                    r0 = s_ * 512 + (p_ % 4) * 128
                    S.dma(lb_[:, s_, :], cout_h[p_ // 4][r0:r0 + 128, 0:129], reads=["cout_h%d" % (p_ // 4)], writes=[lbk])
                order = [0, 1, 2, 3] if d_ == 0 else [3, 2, 1, 0]
                for s_ in order:
                    mcol = (0 if d_ == 0 else 4) + s_
                    sm, smk = small.next()
                    B.ts("dve", sm[:, 0:1], lb_[:, s_, 128:129], segm[:, mcol:mcol + 1], segm[:, 8 + mcol:9 + mcol],
                         ALU.mult, ALU.add, [lbk, "segm"], [smk])
                    t1, t1k = tmp.next()
                    B.ts("dve", t1[:, :128], lb_[:, s_, 0:128], segm[:, mcol:mcol + 1], None, ALU.mult, None,
                         [lbk, "segm"], [t1k])
                    S_n, S_nk = srun.next()
                    B.stt("dve", S_n[:], S_cur[:], sm[:, 0:1], t1[:, :128], ALU.mult, ALU.add, [S_k, smk, t1k], [S_nk])
                    S_cur, S_k = S_n, S_nk
            if not isctx:
                first_lat = False
            psF, psFk = (PS[1], "ps1") if d_ == 0 else (PS[2], "ps2")
            proj_fm(psF, psFk, wf, wfk, c0, cw)
            t_sig, k_sig = tmp.next()
            B.act(t_sig[:, :cw], psF[:, :cw], AF.Sigmoid, [psFk], [k_sig])
            t_lf, k_lf = tmp.next()
            B.act(t_lf[:, :cw], t_sig[:, :cw], AF.Ln, [k_sig, "oml", "lb"], [k_lf],
                  scale=oml[:, hd:hd + 1], bias=lb[:, hd:hd + 1])
            t_kk, k_kk = tmp.next()
            B.ts("dve", t_kk[:, :cw], t_sig[:, :cw], noml[:, hd:hd + 1], oml[:, hd:hd + 1], ALU.mult, ALU.add,
                 [k_sig, "noml", "oml"], [k_kk])
            t_b, k_b = tmp.next()
            S.op("dve", lambda e, o=t_b[:, :cw], a=scanm[:, :cw], b_=t_lf[:, :cw]:
                 e.tensor_tensor_scan(out=o, data0=a, data1=b_, initial=0.0, op0=ALU.mult, op1=ALU.add),
                 reads=["scanm", k_lf], writes=[k_b])
            b3 = t_b[:, :cw].rearrange("p (c s) -> p c s", s=64)
            tot, totk = small.next()
            B.cp("dve", tot[:, :nch], b3[:, :, 63], [k_b], [totk])
            tot_bc = tot[:, :nch].unsqueeze(2).broadcast_to([128, nch, 64])
            if d_ == 1:
                t_c, k_c = tmp.next()
                c3 = t_c[:, :cw].rearrange("p (c s) -> p c s", s=64)
                B.tt("dve", c3, tot_bc, b3, ALU.subtract, [totk, k_b], [k_c])
                B.tt("dve", t_c[:, :cw], t_c[:, :cw], t_lf[:, :cw], ALU.add, [k_c, k_lf], [k_c])
            else:
                t_c, k_c, c3 = t_b, k_b, b3
            t_d3, k_d3 = tmp.next()
            d33 = t_d3[:, :cw].rearrange("p (c s) -> p c s", s=64)
            B.tt("dve", d33, tot_bc, c3, ALU.subtract, [totk, k_c], [k_d3])
            B.act(t_d3[:, :cw], t_d3[:, :cw], AF.Exp, [k_d3], [k_d3])
            kd, kdk = kdr.next()
            B.tt("pool", kd[:, :cw], t_kk[:, :cw], t_d3[:, :cw], ALU.mult, [k_kk, k_d3], [kdk])
            Dc, Dck = small.next()
            B.act(Dc[:, :nch], tot[:, :nch], AF.Exp, [totk], [Dck])
            if not full:
                B.S.op("dve", lambda e, o=totacc[:, ti:ti + 1], i_=tot[:, :nch]:
                       e.tensor_reduce(out=o, in_=i_, axis=AX.X, op=ALU.add), reads=[totk], writes=["totacc"])
            if full:
                cmid_bc = c3[:, :, 32:33].broadcast_to([128, nch, 64])
                t_d1, k_d1 = tmp.next()
                d13 = t_d1[:, :cw].rearrange("p (c s) -> p c s", s=64)
                B.tt("dve", d13, c3, cmid_bc, ALU.subtract, [k_c], [k_d1])
                t_e1, k_e1 = tmp.next()
                B.act(t_e1[:, :cw], t_d1[:, :cw], AF.Exp, [k_d1], [k_e1])
                qb, qbk = qbr.next()
                B.tt("pool", qb[:, :cw], q_sb[:, c0:c0 + cw], t_e1[:, :cw], ALU.mult, ["q", k_e1], [qbk])
                t_e2, k_e2 = t_d1, k_d1
                B.act(t_e2[:, :cw], t_d1[:, :cw], AF.Exp, [k_d1], [k_e2], scale=-1.0)
                kb, kbk = kbr.next()
                B.tt("pool", kb[:, :cw], t_kk[:, :cw], t_e2[:, :cw], ALU.mult, [k_kk, k_e2], [kbk])
                ebr, ebrk = small.next()
                B.act(ebr[:, :nch], c3[:, :, 32], AF.Exp, [k_c], [ebrk])
            if HSTOP <= 1:
                continue
            kdT, kdTk = kdTr.next()
            for sj in range(nsub):
                B.tr(PS4b[:, sj * 128:(sj + 1) * 128], kd[:, sj * 128:(sj + 1) * 128], ident_b[:], [kdk, "identb"], ["ps4"])
            for half in range(2):
                pr = slice(half * 64, (half + 1) * 64)
                B.cp("dve", kdT[pr, half, :nsub, :], PS4b[pr, :nsub * 128].rearrange("p (j k) -> p j k", k=128),
                     ["ps4"], [kdTk])
            if HSTOP <= 2:
                continue
            tj0 = c0 // 128
            ubank = []
            for ch in range(nch):
                sj, half = ch // 2, ch % 2
                UB = [int(v) for v in os.environ.get("UB", "5,6").split(",")]
                bank, bk = (PS[UB[0]], "ps%d" % UB[0]) if half == 0 else (PS[UB[1]], "ps%d" % UB[1])
                B.mm(bank[:, sj * 128:(sj + 1) * 128], kdT[:, half, sj, :],
                     v_tok[:, tj0 + sj, :], True, True, [kdTk, "vtok"], [bk + "_%d" % sj])
                ubank.append((bank[:, sj * 128:(sj + 1) * 128], bk + "_%d" % sj))
            if HSTOP <= 3:
                continue
            if True:
                Usb, Usbk = usbr.next()
                UB = [int(v) for v in os.environ.get("UB", "5,6").split(",")]
                for half in range(2):
                    B.cp("dve" if half == 0 else "act", Usb[:, half, :nsub, :],
                         PS[UB[half]][:, :nsub * 128].rearrange("p (j k) -> p j k", k=128),
                         ["ps%d_%d" % (UB[half], j_) for j_ in range(nsub)], [Usbk + "_%d" % half])
                ubank = [(Usb[:, ch % 2, ch // 2, :], Usbk + "_%d" % (ch % 2)) for ch in range(nch)]
            Sq, Sqk = sqr_.next()
            chs = list(range(nch)) if d_ == 0 else list(range(nch))[::-1]
            CHN = int(os.environ.get("CHN", "99"))
            CHMODE = int(os.environ.get("CHMODE", "3"))
            for ch in chs[:CHN]:
                if full:
                    B.ts("pool", Sq[:, ch, :], S_cur[:], ebr[:, ch:ch + 1], None, ALU.mult, None, [S_k, ebrk], [Sqk + "_%d" % ch])
                S_n, S_nk = srun.next()
                if CHMODE & 1:
                    B.ts("dve", S_n[:], S_cur[:], Dc[:, ch:ch + 1], None, ALU.mult, None, [S_k, Dck], [S_nk])
                if CHMODE & 2:
                    B.tt("dve", S_n[:], ubank[ch][0], S_n[:], ALU.add, [ubank[ch][1], S_nk], [S_nk])
                if CHMODE & 4:
                    B.cp("act", S_n[:], ubank[ch][0], [ubank[ch][1]], [S_nk])
                if CHMODE & 8:
                    B.cp("dve", S_n[:], ubank[ch][0], [ubank[ch][1]], [S_nk])
                S_cur, S_k = S_n, S_nk
            if full and HFULL >= 1:
                mask_ap = masks[:, d_, :]
                for sj in range(nsub):
                    cs = slice(sj * 128, (sj + 1) * 128)
                    B.mm(PS[3][:, cs], kb[:, cs], qb[:, cs], True, True, [kbk, qbk], ["ps3"])
                aTs = []
                for sj in range(nsub):
                    cs = slice(sj * 128, (sj + 1) * 128)
                    aT, aTk = aTr.next()
                    S.op("dve", lambda e, o=aT[:], m_=mask_ap, d__=PS[3][:, cs]: e.copy_predicated(out=o, mask=m_, data=d__),
                         reads=["ps3", "masks"], writes=[aTk])
                    aTs.append((aT, aTk))
                for sj in range(nsub):
                    cs = slice(sj * 128, (sj + 1) * 128)
                    aT, aTk = aTs[sj]
                    B.mm(PS[7][:, cs], v_tok[:, tj0 + sj, :], aT[:], True, False, ["vtok", aTk], ["ps7"])
                    for half in range(2):
                        ch = sj * 2 + half
                        cc = slice(ch * 64, (ch + 1) * 64)
                        B.mm(PS[7][:, cc], Sq[:, ch, :], qb[:, cc], False, half == 1, [Sqk + "_%d" % ch, qbk], ["ps7"])
                if d_ == 0:
                    B.cp("act", o_acc[:, c0:c0 + cw], PS[7][:, :cw], ["ps7"], ["oacc%d" % (c0 // 512)])
                else:
                    B.tt("dve", o_acc[:, c0:c0 + cw], o_acc[:, c0:c0 + cw], PS[7][:, :cw], ALU.add,
                         ["ps7", "oacc%d" % (c0 // 512)], ["oacc%d" % (c0 // 512)])
            ti += 1
        return S_cur, S_k

    for hd in range({14: 0, 16: 1}.get(stage, 8)):
        wv, wvk = load_w(24 + hd)
        wfs = [load_w(8 + hd), load_w(16 + hd)]
        for tj in range(NL // 128):
            bank_col = (tj % 4) * 128
            for k in range(KD):
                B.mm(PS[3][:, bank_col:bank_col + 128], h_sb[:, k, tj * 128:(tj + 1) * 128], wv[:, k, :],
                     k == 0, k == KD - 1, ["h%d" % k, wvk], ["ps3"])
            if tj % 4 == 3:
                B.cp("act", v_tok[:, tj - 3:tj + 1, :], PS[3][:].rearrange("p (j v) -> p j v", v=128), ["ps3"], ["vtok"])
        for d_ in range(2):
            S_f, S_fk = hgrn_dir(hd, d_, False, wfs[d_][0], wfs[d_][1])
            if HSTOP <= 4:
                continue
            p_ = hd * 2 + d_
            ci, r0 = p_ // 4, (p_ % 4) * 128
            S.dma(cin_h[ci][r0:r0 + 128, 0:128], S_f[:], reads=[S_fk], writes=["cin_h%d" % ci], slot="cin_hs%d" % (d_))
            sm, smk = small.next()
            S.op("dve", lambda e, o=sm[:, 0:1], i_=totacc[:, 0:4]: e.tensor_reduce(out=o, in_=i_, axis=AX.X, op=ALU.add),
                 reads=["totacc"], writes=[smk])
            B.act(sm[:, 1:2], sm[:, 0:1], AF.Exp, [smk], [smk])
            S.dma(cin_h[ci][r0:r0 + 128, 128:129], sm[:, 1:2], reads=[smk], writes=["cin_h%d" % ci], slot="cin_hd%d" % (d_),
                  allow_slow_non_contiguous=True)
    import os
    if not os.environ.get("NOCC"):
        for ci in range(4):
            S.coll(lambda e, ci=ci: e.collective_compute("AllGather", ALU.bypass, replica_groups=[[0, 1, 2, 3], [4, 5, 6, 7]],
                                                        ins=[cin_h[ci]], outs=[cout_h[ci]]),
                 reads=["cin_h%d" % ci], writes=["cout_h%d" % ci])

    if stage in (14, 15, 16):
        return finish("x")
    for hd in range(8):
        wq, wqk = load_w(hd)
        wg, wgk = load_w(32 + hd)
        wv, wvk = load_w(24 + hd)
        wfs = [load_w(8 + hd), load_w(16 + hd)]
        S.dma(wo_st[:], hw_out_d[hd * 128:(hd + 1) * 128, :], writes=["wost"])
        B.cp("pool", wo_bf[:], wo_st[:], ["wost"], ["wobf"])
        for (c0, cw, isctx) in col_tiles():
            proj_fm(PS[0], "ps0", wq, wqk, c0, cw)
            B.act(q_sb[:, c0:c0 + cw], PS[0][:, :cw], AF.Silu, ["ps0"], ["q"])
            proj_fm(PS[0], "ps0", wg, wgk, c0, cw)
            B.act(g_sb[:, c0:c0 + cw], PS[0][:, :cw], AF.Silu, ["ps0"], ["g"])
        for tj in range(NT // 128):
            bank_col = (tj % 4) * 128
            for k in range(KD):
                B.mm(PS[3][:, bank_col:bank_col + 128], h_sb[:, k, tj * 128:(tj + 1) * 128], wv[:, k, :],
                     k == 0, k == KD - 1, ["h%d" % k, wvk], ["ps3"])
            if tj % 4 == 3 or tj == NT // 128 - 1:
                n_ = tj % 4 + 1
                B.cp("act", v_tok[:, tj - n_ + 1:tj + 1, :], PS[3][:, :n_ * 128].rearrange("p (j v) -> p j v", v=128),
                     ["ps3"], ["vtok"])
        for d_ in range(2):
            hgrn_dir(hd, d_, True, wfs[d_][0], wfs[d_][1])
        for (c0, cw, isctx) in (col_tiles() if HFULL >= 2 else []):
            col = 1 if isctx else 0
            ok = "oacc%d" % (c0 // 512)
            sq, sqk = qbr.next()
            B.act(sq[:, :cw], o_acc[:, c0:c0 + cw], AF.Square, [ok], [sqk])
            B.mm(PS[1][:, :cw], m128_b[:], sq[:, :cw], True, True, [sqk, "m128b"], ["ps1"])
            rs, rsk = tmp.next()
            B.rsqrt(rs[:, :cw], PS[1][:, :cw], ["ps1"], rsk)
            t1, t1k = tmp.next()
            B.tt("dve", t1[:, :cw], o_acc[:, c0:c0 + cw], rs[:, :cw], ALU.mult, [ok, rsk], [t1k])
            on, onk = onr.next()
            B.stt("dve", on[:, :cw], t1[:, :cw], vecs[:, 56:57], g_sb[:, c0:c0 + cw], ALU.mult, ALU.mult,
                  [t1k, "vecs", "g"], [onk])
            for dk in range(KD):
                bank, bk = (PS[0], "ps0") if dk % 2 == 0 else (PS[2], "ps2")
                B.mm(bank[:, :cw], wo_bf[:, dk * 128:(dk + 1) * 128], on[:, :cw], True, True, ["wobf", onk], [bk])
                B.stt("dve", x_sb[:, dk, c0:c0 + cw], bank[:, :cw], modT[:, 0, 16 + dk, col:col + 1],
                      x_sb[:, dk, c0:c0 + cw], ALU.mult, ALU.add, [bk, "modT", "x%d" % dk], ["x%d" % dk])
    B.release(mh)

    if stage == 2:
        return finish("x")

    def moe_layer(L, with_ctx):
        n = 2 * L + 1
        norm_mod(n, with_ctx)
        mk = B.mark()
        cm2 = B.sb([128, 18, 128], F32, "cm2")
        S.dma(cm2[:], cm2_d, writes=["cm2"])
        tiles = col_tiles(with_ctx)
        ncols = NT if with_ctx else NL
        gw = B.sb([128, NT], F32, "gw")
        mk2 = B.mark()
        wr_st = B.sb([128, KD, NE], F32, "wrst")
        wr_bf = B.sb([128, KD, 128], BF16, "wrbf")
        S.dma(wr_st[:], router_d[L].rearrange("(k p) e -> p k e", p=128), writes=["wrst"])
        B.memset("pool", wr_bf[:], 0.0, ["wrbf"])
        B.cp("pool", wr_bf[:, :, 0:NE], wr_st[:], ["wrst", "wrbf"], ["wrbf"])
        etile = B.ring(2, [128, 512], F32, "etile")
        for (c0, cw, isctx) in tiles:
            for k in range(KD):
                B.mm(PS[0][:, :cw], wr_bf[:, k, :], h_sb[:, k, c0:c0 + cw], k == 0, k == KD - 1, ["wrbf", "h%d" % k], ["ps0"])
            et, etk = etile.next()
            B.act(et[:, :cw], PS[0][:, :cw], AF.Exp, ["ps0"], [etk])
            B.mm(PS[1][:, :cw], cm2[:, 16, :], et[:, :cw], True, True, ["cm2", etk], ["ps1"])
            rt, rtk = etile.next()
            S.op("dve", lambda e, o=rt[:, :cw], i_=PS[1][:, :cw]: e.reciprocal(out=o, in_=i_), reads=["ps1"], writes=[rtk])
            B.tt("dve", gw[:, c0:c0 + cw], et[:, :cw], rt[:, :cw], ALU.mult, [etk, rtk], ["gw"])
        cin_a = B.dram_tmp("cin_a%d" % L, [NE, NL])
        cout_a = B.dram_tmp("cout_a%d" % L, [4 * NE, NL])
        S.dma(cin_a, gw[0:NE, 0:NL], reads=["gw"], writes=["cin_a"])
        S.coll(lambda e: e.collective_compute("AllGather", ALU.bypass, replica_groups=[[0, 1, 2, 3], [4, 5, 6, 7]],
                                              ins=[cin_a], outs=[cout_a]), reads=["cin_a"], writes=["cout_a"])
        cin_z = B.dram_tmp("cin_z%d" % L, [NE, 64])
        cout_z = B.dram_tmp("cout_z%d" % L, [4 * NE, 64])
        S.dma(cin_z, gw[0:NE, 0:64], reads=["gw", "cout_a"], writes=["cin_z"])
        S.coll(lambda e: e.collective_compute("AllGather", ALU.bypass, replica_groups=[[0, 1, 2, 3], [4, 5, 6, 7]],
                                              ins=[cin_z], outs=[cout_z]), reads=["cin_z", "cout_a"], writes=["cout_z", "cout_a"])
        A = B.sb([128, NL], F32, "A")
        B.memset("pool", A[:], 0.0, ["A"])
        S.dma(A[0:64, :], cout_a, reads=["cout_a", "A"], writes=["A"])
        if with_ctx:
            S.dma(A[64:80, 0:NCX], gw[0:NE, NL:NT], reads=["gw", "A"], writes=["A"])
        msk = B.sb([128, NL], F32, "msk")
        thr = B.sb([128, 1], F32, "thr")
        B.memset("pool", thr[:], 0.0, ["thr"])
        sm_ = B.ring(4, [128, 2], F32, "thsm")
        sm8 = B.ring(2, [128, 8], F32, "thsm8")
        for it in range(1, 31):
            step = 2.0 ** (-it)
            cand, ck = sm_.next()
            B.ts("dve", cand[:, 0:1], thr[:], step, None, ALU.add, None, ["thr"], [ck])
            B.ts("dve", msk[:], A[:], cand[:, 0:1], None, ALU.is_ge, None, ["A", ck], ["msk"])
            c8, c8k = sm8.next()
            S.op("dve", lambda e, o=c8[:]: e.tensor_reduce(out=o, in_=msk[:].rearrange("p (g c) -> p g c", c=256),
                                                          axis=AX.X, op=ALU.add), reads=["msk"], writes=[c8k])
            B.mm(PS[2][:, 0:8], cm2[:, 17, :], c8[:], True, True, ["cm2", c8k], ["ps2"])
            S.op("dve", lambda e, o=cand[:, 1:2]: e.tensor_reduce(out=o, in_=PS[2][:, 0:8], axis=AX.X, op=ALU.add),
                 reads=["ps2"], writes=[ck])
            ge, gk = sm_.next()
            B.tt("dve", ge[:, 0:1], cand[:, 1:2], vecs[:, 58:59], ALU.is_ge, [ck, "vecs"], [gk])
            B.stt("dve", thr[:], ge[:, 0:1], step, thr[:], ALU.mult, ALU.add, [gk, "thr"], ["thr"])
        B.stt("dve", gw[0:NE, 0:NL], gw[0:NE, 0:NL], thr[0:NE, 0:1], gw[0:NE, 0:NL], ALU.is_ge, ALU.mult, ["gw", "thr"], ["gw"])
        if with_ctx:
            thc = B.sb([128, 1], F32, "thc")
            S.dma(thc[0:NE, 0:1], thr[64:80, 0:1], reads=["thr"], writes=["thc"])
            B.stt("dve", gw[0:NE, NL:NT], gw[0:NE, NL:NT], thc[0:NE, 0:1], gw[0:NE, NL:NT], ALU.is_ge, ALU.mult,
                  ["gw", "thc"], ["gw"])
        B.ts("pool", gw[:], gw[:], cm2[:, 16, 0:1], None, ALU.mult, None, ["gw", "cm2"], ["gw"])
        B.release(mk2)
        gwbr = B.ring(2, [128, NT], F32, "gwb")
        wgs = B.ring(2, [128, KD, 128], F32, "wgs")
        wus = B.ring(2, [128, KD, 128], F32, "wus")
        wds = B.ring(2, [128, D], F32, "wds")
        wgb = B.ring(2, [128, KD, 128], BF16, "wgb")
        wub = B.ring(2, [128, KD, 128], BF16, "wub")
        wdb = B.ring(2, [128, D], BF16, "wdb")
        sar = B.ring(2, [128, 512], F32, "sa")
        actr = B.ring(2, [128, 512], BF16, "actb")
        ysr = B.ring(3, [128, 512], F32, "ysb")
        psa = Ring([PS[0], PS[1]], "psA")
        psu = Ring([PS[2], PS[3]], "psU")
        psy = Ring([PS[4], PS[5], PS[6], PS[7]], "psY")
        NACT = 3
        NEXP = int(os.environ.get("NEXP", str(NE)))
        for e_ in range(NEXP):
            gwb, gwbk = gwbr.next()
            for (c0, cw, isctx) in tiles:
                pa, pak = psa.next()
                B.mm(pa[:, :cw], cm2[:, e_, :], gw[:, c0:c0 + cw], True, True, ["cm2", "gw"], [pak])
                B.cp("act", gwb[:, c0:c0 + cw], pa[:, :cw], [pak], [gwbk])
            for fb in range(NF):
                g_s, g_sk = wgs.next(); u_s, u_sk = wus.next(); d_s, d_sk = wds.next()
                S.dma(g_s[:], wgate_d[L, e_, :, fb * 128:(fb + 1) * 128].rearrange("(k p) f -> p k f", p=128), writes=[g_sk])
                S.dma(u_s[:], wup_d[L, e_, :, fb * 128:(fb + 1) * 128].rearrange("(k p) f -> p k f", p=128), writes=[u_sk])
                S.dma(d_s[:], wdown_d[L, e_, fb * 128:(fb + 1) * 128, :], writes=[d_sk])
                g_b, g_bk = wgb.next(); u_b, u_bk = wub.next(); d_b, d_bk = wdb.next()
                B.cp("pool", g_b[:], g_s[:], [g_sk], [g_bk])
                B.cp("pool", u_b[:], u_s[:], [u_sk], [u_bk])
                B.cp("pool", d_b[:], d_s[:], [d_sk], [d_bk])
                for (c0, cw, isctx) in tiles:
                    col = 1 if isctx else 0
                    pa, pak = psa.next(); pu, puk = psu.next()
                    for k in range(KD):
                        B.mm(pa[:, :cw], g_b[:, k, :], h_sb[:, k, c0:c0 + cw], k == 0, k == KD - 1, [g_bk, "h%d" % k], [pak])
                    for k in range(KD):
                        B.mm(pu[:, :cw], u_b[:, k, :], h_sb[:, k, c0:c0 + cw], k == 0, k == KD - 1, [u_bk, "h%d" % k], [puk])
                    sa, sak = sar.next()
                    B.act(sa[:, :cw], pa[:, :cw], AF.Silu, [pak], [sak])
                    B.tt("dve", sa[:, :cw], sa[:, :cw], pu[:, :cw], ALU.mult, [sak, puk], [sak])
                    ab, abk = actr.next()
                    B.tt("pool", ab[:, :cw], sa[:, :cw], gwb[:, c0:c0 + cw], ALU.mult, [sak, gwbk], [abk])
                    for dk in range(KD):
                        py, pyk = psy.next()
                        B.mm(py[:, :cw], d_b[:, dk * 128:(dk + 1) * 128], ab[:, :cw], True, True, [d_bk, abk], [pyk])
                        gsc = modT[:, L, 40 + dk, col:col + 1]
                        if dk < NACT:
                            ys, ysk = ysr.next()
                            B.act(ys[:, :cw], py[:, :cw], AF.Identity, [pyk, "modT"], [ysk], scale=gsc)
                            B.tt("pool", x_sb[:, dk, c0:c0 + cw], x_sb[:, dk, c0:c0 + cw], ys[:, :cw], ALU.add,
                                 ["x%d" % dk, ysk], ["x%d" % dk])
                        else:
                            B.stt("dve", x_sb[:, dk, c0:c0 + cw], py[:, :cw], gsc, x_sb[:, dk, c0:c0 + cw],
                                  ALU.mult, ALU.add, [pyk, "modT", "x%d" % dk], ["x%d" % dk])
        B.release(mk)

    moe_layer(0, True)
    if stage == 3:
        return finish("x")

    LAM_INIT = 0.8 - 0.6 * math.exp(-0.3 * 1)
    norm_mod(2, True)
    ma = B.mark()
    q_all = B.sb([128, 8, NL], BF16, "qall")
    kctx = B.sb([128, 8, NCX], BF16, "kctx")
    vctx = B.sb([128, 8, 2, 128], BF16, "vctx")
    nlam = B.sb([128, 4], F32, "nlam")
    mlam = B.mark()
    lvb = B.sb([128, 256], F32, "lvb")
    S.dma(lvb[:], lvb_d, writes=["lvb"])
    lvp = B.sb([128, 128], F32, "lvp")
    B.tt("dve", lvp[:, 0:64], lvb[:, 0:64], lvb[:, 64:128], ALU.mult, ["lvb"], ["lvp"])
    B.tt("dve", lvp[:, 64:128], lvb[:, 128:192], lvb[:, 192:256], ALU.mult, ["lvb"], ["lvp"])
    S.op("dve", lambda e: e.tensor_reduce(out=nlam[:, 2:3], in_=lvp[:, 0:64], axis=AX.X, op=ALU.add), reads=["lvp"], writes=["nlam"])
    S.op("dve", lambda e: e.tensor_reduce(out=nlam[:, 3:4], in_=lvp[:, 64:128], axis=AX.X, op=ALU.add), reads=["lvp"], writes=["nlam"])
    B.act(nlam[:, 2:4], nlam[:, 2:4], AF.Exp, ["nlam"], ["nlam"])
    B.tt("dve", nlam[:, 0:1], nlam[:, 3:4], nlam[:, 2:3], ALU.subtract, ["nlam"], ["nlam"])
    B.ts("dve", nlam[:, 0:1], nlam[:, 0:1], -LAM_INIT, None, ALU.add, None, ["nlam"], ["nlam"])
    B.ts("dve", nlam[:, 1:2], vecs[:, 57:58], 1.0 - LAM_INIT, None, ALU.mult, None, ["vecs"], ["nlam"])
    B.release(mlam)

    cin_k = [B.dram_tmp("cin_k%d" % i, [128, NL], BF16) for i in range(8)]
    cout_k = [B.dram_tmp("cout_k%d" % i, [512, NL], BF16) for i in range(8)]
    cin_v = [B.dram_tmp("cin_v%d" % i, [NL, 128], BF16) for i in range(8)]
    cout_v = [B.dram_tmp("cout_v%d" % i, [4 * NL, 128], BF16) for i in range(8)]

    mpa = B.mark()
    rope = B.sb([128, 2, NL], F32, "rope")
    S.dma(rope[:], rope_d, writes=["rope"])
    wst2 = B.ring(2, [128, KD, 128], F32, "wst2")
    wbf2 = B.ring(3, [128, KD, 128], BF16, "wbf2")
    rtmp = B.ring(4, [128, 512], F32, "rtmp")
    kT_loc = B.ring(1, [128, NL], BF16, "kTloc")
    v_loc = B.ring(1, [128, NL // 128, 128], BF16, "vloc")

    def load_w2(j):
        st_, stk = wst2.next()
        S.dma(st_[:], dw_in_d[:, j * 128:(j + 1) * 128].rearrange("(k p) c -> p k c", p=128), writes=[stk])
        wb, wbk = wbf2.next()
        B.cp("pool", wb[:], st_[:], [stk], [wbk])
        return wb, wbk

    def rope_tile(ps, psk, c0, out_ap, outk):
        xf, xfk = rtmp.next()
        B.cp("act", xf[:], ps[:], [psk], [xfk])
        B.mm(PS[2][:], cmat[:, 2, :], xf[:], True, True, ["cmat", xfk], ["ps2"])
        t1, t1k = rtmp.next()
        B.tt("pool", t1[:], xf[:], rope[:, 0, c0:c0 + 512], ALU.mult, [xfk, "rope"], [t1k])
        t2, t2k = rtmp.next()
        B.tt("dve", t2[:], PS[2][:], rope[:, 1, c0:c0 + 512], ALU.mult, ["ps2", "rope"], [t2k])
        B.tt("dve", out_ap, t1[:], t2[:], ALU.add, [t1k, t2k], [outk])

    for hd in range(8):
        wq, wqk = load_w2(hd)
        wk_, wkk = load_w2(8 + hd)
        wv, wvk = load_w2(16 + hd)
        kT, kTk = kT_loc.next()
        for (c0, cw, isctx) in col_tiles(True):
            if not isctx:
                proj_fm(PS[0], "ps0", wq, wqk, c0, cw)
                rope_tile(PS[0], "ps0", c0, q_all[:, hd, c0:c0 + cw], "qall%d" % hd)
                proj_fm(PS[1], "ps1", wk_, wkk, c0, cw)
                rope_tile(PS[1], "ps1", c0, kT[:, c0:c0 + cw], kTk)
            else:
                proj_fm(PS[1], "ps1", wk_, wkk, c0, cw)
                B.cp("act", kctx[:, hd, :], PS[1][:, :cw], ["ps1"], ["kctx"])
        S.dma(cin_k[hd], kT[:], reads=[kTk], writes=["cin_k%d" % hd])
        S.coll(lambda e, hd=hd: e.collective_compute("AllGather", ALU.bypass, replica_groups=[[0, 1, 2, 3], [4, 5, 6, 7]],
                                                      ins=[cin_k[hd]], outs=[cout_k[hd]]),
             reads=["cin_k%d" % hd], writes=["cout_k%d" % hd])
        vl, vlk = v_loc.next()
        for tj in range(NT // 128):
            bank_col = (tj % 4) * 128
            for k in range(KD):
                B.mm(PS[3][:, bank_col:bank_col + 128], h_sb[:, k, tj * 128:(tj + 1) * 128], wv[:, k, :],
                     k == 0, k == KD - 1, ["h%d" % k, wvk], ["ps3"])
            if tj % 4 == 3 and tj < 16:
                B.cp("act", vl[:, tj - 3:tj + 1, :], PS[3][:].rearrange("p (j v) -> p j v", v=128), ["ps3"], [vlk])
            if tj == 17:
                B.cp("act", vctx[:, hd, :, :], PS[3][:, 0:256].rearrange("p (j v) -> p j v", v=128), ["ps3"], ["vctx"])
        S.dma(cin_v[hd].rearrange("(j p) v -> p j v", p=128), vl[:], reads=[vlk], writes=["cin_v%d" % hd])
        S.coll(lambda e, hd=hd: e.collective_compute("AllGather", ALU.bypass, replica_groups=[[0, 1, 2, 3], [4, 5, 6, 7]],
                                                      ins=[cin_v[hd]], outs=[cout_v[hd]]),
             reads=["cin_v%d" % hd], writes=["cout_v%d" % hd])
    B.release(mpa)
    if stage == 35:
        return finish("x")

    NKT = (4 * NL + NCX) // 128
    h_off = B.SB_BASE + 128 * 0 + KD * NT * 4
    K01 = nc.alloc_sbuf_tensor_at("K01", [128, 2, NKT * 128], BF16, offset=h_off)
    B.memset("pool", K01[64:128, 0, :], 0.0, ["K01"])
    B.memset("pool", K01[0:64, 1, :], 0.0, ["K01"])
    V_h = B.sb([128, NKT, 128], BF16, "Vh")
    er = B.ring(4, [128, 512], BF16, "etl")
    ftmp = B.ring(6, [128, 512], F32, "ftmp")
    onr2 = B.ring(2, [128, 512], BF16, "on2")
    wo_st2 = B.sb([128, D], F32, "wost2")
    wo_bf2 = B.sb([128, D], BF16, "wobf2")
    psS = [Ring([PS[0], PS[1]], "psS0"), Ring([PS[2], PS[3]], "psS1")]
    NKT_RUN = int(os.environ.get("NKT", str(NKT)))
    for hd in range(8):
        for r_ in range(4):
            for c_ in range(2):
                S.dma(K01[c_ * 64:(c_ + 1) * 64, c_, r_ * NL:(r_ + 1) * NL],
                      cout_k[hd][r_ * 128 + c_ * 64:r_ * 128 + (c_ + 1) * 64, :], reads=["cout_k%d" % hd, "K01"], writes=["K01"])
            S.dma(V_h[:, r_ * 16:(r_ + 1) * 16, :], cout_v[hd][r_ * NL:(r_ + 1) * NL, :].rearrange("(j p) v -> p j v", p=128),
                  reads=["cout_v%d" % hd, "Vh"], writes=["Vh"])
        for c_ in range(2):
            B.cp("pool", K01[c_ * 64:(c_ + 1) * 64, c_, 4 * NL:4 * NL + NCX], kctx[c_ * 64:(c_ + 1) * 64, hd, :], ["kctx", "K01"], ["K01"])
        B.cp("pool", V_h[:, 64:66, :], vctx[:, hd, :, :], ["vctx", "Vh"], ["Vh"])
        S.dma(wo_st2[:], dw_out_d[hd * 128:(hd + 1) * 128, :], writes=["wost2"])
        B.cp("pool", wo_bf2[:], wo_st2[:], ["wost2"], ["wobf2"])
        for qt in range(NL // 512):
            qc = slice(qt * 512, (qt + 1) * 512)
            for kt in range(NKT_RUN):
                kc = slice(kt * 128, (kt + 1) * 128)
                first, last = kt == 0, kt == NKT_RUN - 1
                for c_ in range(2):
                    ps_, psk = psS[c_].next()
                    B.mm(ps_[:], K01[:, c_, kc], q_all[:, hd, qc], True, True, ["K01", "qall%d" % hd], [psk])
                    et, etk = er.next()
                    B.act(et[:], ps_[:], AF.Exp, [psk], [etk], scale=DIFF_SCALE)
                    B.mm(PS[4 + c_][:], V_h[:, kt, :], et[:], first, last, ["Vh", etk], ["ps%d" % (4 + c_)])
                    B.mm(PS[6 + c_][:], ones_b[:], et[:], first, last, ["onesb", etk], ["ps%d" % (6 + c_)])
            oc = []
            for c_ in range(2):
                rc, rck = ftmp.next()
                S.op("dve", lambda e, o=rc[:], i_=PS[6 + c_][:]: e.reciprocal(out=o, in_=i_), reads=["ps%d" % (6 + c_)], writes=[rck])
                B.tt("dve", rc[:], PS[4 + c_][:], rc[:], ALU.mult, ["ps%d" % (4 + c_), rck], [rck])
                oc.append((rc, rck))
            o_, ok_ = ftmp.next()
            B.stt("dve", o_[:], oc[1][0][:], nlam[:, 0:1], oc[0][0][:], ALU.mult, ALU.add, [oc[1][1], oc[0][1], "nlam"], [ok_])
            sq, sqk = er.next()
            B.act(sq[:], o_[:], AF.Square, [ok_], [sqk])
            ps_, psk = psS[0].next()
            B.mm(ps_[:], m128_b[:], sq[:], True, True, [sqk, "m128b"], [psk])
            rs, rsk = ftmp.next()
            B.rsqrt(rs[:], ps_[:], [psk], rsk)
            B.tt("dve", o_[:], o_[:], rs[:], ALU.mult, [ok_, rsk], [ok_])
            on, onk = onr2.next()
            B.ts("dve", on[:], o_[:], nlam[:, 1:2], None, ALU.mult, None, [ok_, "nlam"], [onk])
            for dk in range(KD):
                ps_, psk = psS[dk % 2].next()
                B.mm(ps_[:], wo_bf2[:, dk * 128:(dk + 1) * 128], on[:], True, True, ["wobf2", onk], [psk])
                B.stt("dve", x_sb[:, dk, qc], ps_[:], modT[:, 1, 16 + dk, 0:1], x_sb[:, dk, qc], ALU.mult, ALU.add,
                      [psk, "modT", "x%d" % dk], ["x%d" % dk])
    B.release(ma)
    if stage == 4:
        return finish("x")

    moe_layer(1, False)

    mf = B.mark()
    sqr = B.ring(3, [128, 512], BF16, "fsq")
    rsr = B.ring(2, [128, 512], F32, "frs")
    otr = B.ring(3, [128, 512], F32, "fout")
    for (c0, cw, isctx) in col_tiles(False):
        for k in range(KD):
            sq, sqk = sqr.next()
            B.act(sq[:], x_sb[:, k, c0:c0 + cw], AF.Square, ["x%d" % k], [sqk])
            B.mm(PS[1][:], mean_b[:], sq[:], k == 0, k == KD - 1, [sqk, "meanb"], ["ps1"])
        rs, rsk = rsr.next()
        B.rsqrt(rs[:], PS[1][:], ["ps1"], rsk)
        for k in range(KD):
            ot, otk = otr.next()
            B.stt("dve", ot[:], x_sb[:, k, c0:c0 + cw], vecs[:, 32 + k:33 + k], rs[:], ALU.mult, ALU.mult,
                  ["x%d" % k, "vecs", rsk], [otk])
            S.dma(out_d[k * 128:(k + 1) * 128, c0:c0 + cw], ot[:], reads=[otk], writes=["out%d" % k])
    S.wait_all("sp", ["out%d" % k for k in range(KD)])
    if dbg:
        for k in range(KD):
            S.dma(dbg_d[k * 128:(k + 1) * 128, :], x_sb[:, k, :], reads=["x%d" % k], writes=["dbg%d" % k])
        S.wait_all("sp", ["dbg%d" % k for k in range(KD)])
    S.emit()
    return nc

    return finish("x")


def _consts():
    ident = np.eye(128, dtype=np.float32)
    ones = np.ones((128, 128), np.float32)
    PT = np.zeros((128, 128), np.float32)
    for p in range(128):
        j = p % 64
        if j < 32:
            PT[p + 32, p] = -1.0
        else:
            PT[p - 32, p] = 1.0
    cmat = np.stack([ident, ones, PT, np.zeros((128, 128), np.float32)], axis=1)
    s = np.arange(128)[:, None]
    t = np.arange(128)[None, :]
    same = (s // 64) == (t // 64)
    mfw = (same & (s <= t)).astype(np.int32)
    mbw = (same & (s >= t)).astype(np.int32)
    masks = np.stack([mfw, mbw], axis=1)
    scanm = np.ones((128, 512), np.float32)
    scanm[:, ::64] = 0.0
    cm2 = np.zeros((128, 18, 128), np.float32)
    for e in range(16):
        cm2[e, e, :] = 1.0
    cm2[0:16, 16, :] = 1.0
    for p in range(80):
        for q in range(80):
            if (p < 64 and q < 64 and p % 16 == q % 16) or (p >= 64 and q == p):
                cm2[p, 17, q] = 1.0
    _consts.cm2 = cm2
    return np.ascontiguousarray(cmat), np.ascontiguousarray(masks), scanm


def make_in_maps(inputs):
    f = lambda a: np.ascontiguousarray(np.asarray(a, dtype=np.float32))
    x, c, ctx, c_ctx = f(inputs["x"]), f(inputs["c"]), f(inputs["ctx"]), f(inputs["c_ctx"])
    cmat, masks, scanm = _consts()
    vecs = np.zeros((128, 64), np.float32)
    pk = lambda v: np.asarray(v, np.float32).reshape(8, 128).T
    nm, nf = f(inputs["norm_mix"]), f(inputs["norm_ffn"])
    vecs[:, 0:8] = pk(nm[0]); vecs[:, 8:16] = pk(nm[1])
    vecs[:, 16:24] = pk(nf[0]); vecs[:, 24:32] = pk(nf[1])
    vecs[:, 32:40] = pk(f(inputs["norm_final"]))
    lbl = f(inputs["hgrn_lb_logits"])
    vecs[:, 40:48] = pk(lbl[0]); vecs[:, 48:56] = pk(lbl[1])
    vecs[:, 56] = f(inputs["hgrn_norm"])[0]
    vecs[:, 57] = f(inputs["diff_subln"])[0]
    vecs[0:64, 58] = float(CAP_L)
    vecs[64:128, 58] = float(CAP_C)
    adab = np.ascontiguousarray(f(inputs["ada_b"]).reshape(2, 48, 128).transpose(2, 0, 1))
    shared = {
        "ada_w": f(inputs["ada_w"]), "adab": adab, "vecs": vecs, "cmat": cmat, "masks": masks, "scanm": scanm,
        "hgrn_w_in": f(inputs["hgrn_w_in"])[0], "hgrn_w_out": f(inputs["hgrn_w_out"])[0],
        "diff_w_in": f(inputs["diff_w_in"])[0], "diff_w_out": f(inputs["diff_w_out"])[0],
        "lvb": np.ascontiguousarray(np.tile(f(inputs["diff_lambda"])[0].reshape(1, 256), (128, 1))),
        "cm2": _consts.cm2, "moe_router": f(inputs["moe_router"]), "moe_w_gate": f(inputs["moe_w_gate"]),
        "moe_w_up": f(inputs["moe_w_up"]), "moe_w_down": f(inputs["moe_w_down"]),
    }
    in_maps = []
    for core in range(NCORES):
        b, seg = core // 4, core % 4
        xT = np.ascontiguousarray(np.concatenate([x[b, seg * NL:(seg + 1) * NL, :], ctx[b]], axis=0).T)
        cT = np.ascontiguousarray(np.stack([c[b], c_ctx], axis=1))
        segm = np.zeros((128, 16), np.float32)
        for s in range(4):
            segm[:, s] = 1.0 if s < seg else 0.0
            segm[:, 4 + s] = 1.0 if s > seg else 0.0
        segm[:, 8:16] = 1.0 - segm[:, 0:8]
        tg = seg * NL + np.arange(NL)
        freq = (10000.0 ** (-np.arange(16, dtype=np.float32) / 16.0)).astype(np.float32)
        ang = np.concatenate([(tg // 64).astype(np.float32)[:, None] * freq, (tg % 64).astype(np.float32)[:, None] * freq], axis=-1)
        pj = np.arange(128) % 32
        rope = np.ascontiguousarray(np.stack([np.cos(ang)[:, pj].T, np.sin(ang)[:, pj].T], axis=1).astype(np.float32))
        m = dict(shared)
        m.update({"xT": xT, "cT": cT, "segm": segm, "rope": rope})
        in_maps.append(m)
    return in_maps


def run(inputs, stage=99, dbg=False):
    nc = build(stage, dbg)
    in_maps = make_in_maps(inputs)
    import os
    n1 = int(os.environ.get("NC1", "0"))
    if n1:
        return run_bass_kernel_spmd(nc, in_maps[:n1], core_ids=list(range(n1)))
    res = run_bass_kernel_spmd(nc, in_maps, core_ids=list(range(NCORES)))
    return res


def kernel(**inputs):
    res = run(inputs, stage=99)
    out = np.zeros((2, 8192, D), np.float32)
    for core in range(NCORES):
        b, seg = core // 4, core % 4
        out[b, seg * NL:(seg + 1) * NL, :] = res.results[core]["outT"].T
    return out
```

```python
from contextlib import ExitStack
import math
import numpy as np
import ml_dtypes
import concourse.bass as bass
import concourse.mybir as mybir
from concourse.bass_utils import run_bass_kernel_spmd

F32 = mybir.dt.float32
BF16 = mybir.dt.bfloat16
I32 = mybir.dt.int32
AF = mybir.ActivationFunctionType
ALU = mybir.AluOpType
AX = mybir.AxisListType

NCORES = 8
D = 1024
KD = 8
NL = 2048
NCX = 256
NT = NL + NCX
EPS = 1e-6
NE = 16
DEXP = 2816
NF = DEXP // 128
CAP_L = 1024
CAP_C = 32
DIFF_SCALE = 0.125

COMPUTE = ("pe", "act", "dve", "pool")
QUEUES = ("sp",)


class Sched:
    def __init__(self, nc):
        self.nc = nc
        self.streams = {e: [] for e in COMPUTE + QUEUES}
        self.cnt = {}
        self.seen = {e: {} for e in COMPUTE + QUEUES}
        self.last_w = {}
        self.readers = {}
        self.n_ins = 0
        self.n_wait = 0

    def _deps(self, reads, writes):
        deps = {}

        def add(sk, n):
            if deps.get(sk, 0) < n:
                deps[sk] = n

        for r in reads:
            lw = self.last_w.get(r)
            if lw:
                add(*lw)
        for w in writes:
            lw = self.last_w.get(w)
            if lw:
                add(*lw)
            for sk, n in self.readers.get(w, {}).items():
                add(sk, n)
        return deps

    def _emit_waits(self, eng, deps, skip_self=False):
        for sk, n in deps.items():
            if skip_self and sk == eng:
                continue
            if self.seen[eng].get(sk, 0) >= n:
                continue
            self.seen[eng][sk] = n
            self.streams[eng].append(("wait", sk, n))
            self.n_wait += 1

    def _record(self, sk, n, reads, writes):
        for r in reads:
            d = self.readers.setdefault(r, {})
            if d.get(sk, 0) < n:
                d[sk] = n
        for w in writes:
            self.last_w[w] = (sk, n)
            self.readers[w] = {}

    def op(self, eng, fn, reads=(), writes=()):
        deps = self._deps(reads, writes)
        self._emit_waits(eng, deps, skip_self=(eng == "pe"))
        n = self.cnt.get(eng, 0) + 1
        self.cnt[eng] = n
        self.streams[eng].append(("ins", fn, eng, 1))
        self._record(eng, n, reads, writes)
        self.n_ins += 1

    def dma(self, out, in_, reads=(), writes=(), slot=None, q="sp", **kw):
        if slot is None:
            slot = writes[0]
        sk = ("dma", slot)
        deps = self._deps(reads, writes)
        self._emit_waits(q, deps)
        n = self.cnt.get(sk, 0) + 1
        self.cnt[sk] = n
        self.streams[q].append(("ins", lambda e: e.dma_start(out=out, in_=in_, **kw), sk, 16))
        self._record(sk, n, reads, writes)
        self.n_ins += 1

    def coll(self, fn, reads=(), writes=()):
        self.op("pool", fn, reads=reads, writes=writes)

    def wait_all(self, eng, bufkeys):
        self._emit_waits(eng, self._deps(bufkeys, ()))

    def barrier(self):
        snap = dict(self.cnt)
        for eng in COMPUTE + QUEUES:
            self._emit_waits(eng, snap)

    def emit(self):
        nc = self.nc
        semkeys = list(self.cnt.keys())
        assert len(semkeys) <= 100, len(semkeys)
        with ExitStack() as st:
            sems = {}
            for i, sk in enumerate(semkeys):
                sems[sk] = st.enter_context(nc.semaphore("s%d" % i))
            block = st.enter_context(nc.Block())

            def run(eng_name):
                def body(e):
                    for item in self.streams[eng_name]:
                        if item[0] == "wait":
                            _, sk, n = item
                            mult = 16 if isinstance(sk, tuple) else 1
                            e.wait_ge(sems[sk], n * mult)
                        else:
                            _, fn, sk, inc = item
                            fn(e).then_inc(sems[sk], inc)
                return body

            block.tensor(run("pe"))
            block.scalar(run("act"))
            block.vector(run("dve"))
            block.gpsimd(run("pool"))
            block.sync(run("sp"))


class Ring:
    def __init__(self, aps, name):
        self.aps = aps
        self.name = name
        self.i = 0

    def next(self):
        j = self.i % len(self.aps)
        self.i += 1
        return self.aps[j], "%s%d" % (self.name, j)


class Builder:
    SB_BASE = 16512
    SB_TOP = 229344

    def __init__(self, stage, dbg):
        self.stage = stage
        self.dbg = dbg
        self.nc = bass.Bass("TRN2", target_bir_lowering=False)
        self.S = Sched(self.nc)
        self.off = self.SB_BASE
        self.names = 0
        self.ps_banks = []

    def sb(self, shape, dt, name=None):
        esz = 4 if dt in (F32, I32) else 2
        sz = int(np.prod(shape[1:])) * esz
        sz = (sz + 31) // 32 * 32
        self.names += 1
        nm = "%s_%d" % (name or "t", self.names)
        t = self.nc.alloc_sbuf_tensor_at(nm, list(shape), dt, offset=self.off)
        self.off += sz
        assert self.off <= self.SB_TOP, ("SBUF overflow", nm, self.off)
        return t

    def mark(self):
        return self.off

    def release(self, mark):
        self.S.barrier()
        self.off = mark

    def ring(self, n, shape, dt, name):
        return Ring([self.sb(shape, dt, name) for _ in range(n)], name + "_%d_" % self.names)

    def dram_in(self, name, shape, dt=F32):
        return self.nc.dram_tensor(name, list(shape), dt, kind="ExternalInput").ap()

    def dram_out(self, name, shape, dt=F32):
        return self.nc.dram_tensor(name, list(shape), dt, kind="ExternalOutput").ap()

    def dram_tmp(self, name, shape, dt=F32):
        return self.nc.dram_tensor(name, list(shape), dt).ap()

    def mm(self, out, lhsT, rhs, start, stop, r, w):
        self.S.op("pe", lambda e: e.matmul(out, lhsT=lhsT, rhs=rhs, start=start, stop=stop), reads=r, writes=w)

    def tr(self, out, in_, ident, r, w):
        self.S.op("pe", lambda e: e.transpose(out, in_, ident), reads=r, writes=w)

    def act(self, out, in_, func, r, w, scale=1.0, bias=0.0):
        self.S.op("act", lambda e: e.activation(out=out, in_=in_, func=func, bias=bias, scale=scale), reads=r, writes=w)

    def tt(self, eng, out, a, b_, op, r, w):
        self.S.op(eng, lambda e: e.tensor_tensor(out=out, in0=a, in1=b_, op=op), reads=r, writes=w)

    def ts(self, eng, out, a, s1, s2, op0, op1, r, w):
        if s2 is None:
            self.S.op(eng, lambda e: e.tensor_scalar(out=out, in0=a, scalar1=s1, scalar2=None, op0=op0), reads=r, writes=w)
        else:
            self.S.op(eng, lambda e: e.tensor_scalar(out=out, in0=a, scalar1=s1, scalar2=s2, op0=op0, op1=op1), reads=r, writes=w)

    def stt(self, eng, out, in0, scalar, in1, op0, op1, r, w):
        self.S.op(eng, lambda e: e.scalar_tensor_tensor(out=out, in0=in0, scalar=scalar, in1=in1, op0=op0, op1=op1), reads=r, writes=w)

    def cp(self, eng, out, in_, r, w):
        if eng == "act":
            self.S.op("act", lambda e: e.copy(out=out, in_=in_), reads=r, writes=w)
        else:
            self.S.op(eng, lambda e: e.tensor_copy(out=out, in_=in_), reads=r, writes=w)

    def rsqrt(self, out, in_, r, wk, eps=EPS):
        self.act(out, in_, AF.Sqrt, list(r) + ["epsT"], [wk], bias=self.eps_ap(eps))
        self.S.op("dve", lambda e: e.reciprocal(out=out, in_=out), reads=[wk], writes=[wk])

    def eps_ap(self, eps):
        return self.epsT[:, 0:1]

    def memset(self, eng, ap, val, w):
        self.S.op(eng, lambda e: e.memset(ap, val), reads=(), writes=w)


def col_tiles(with_ctx=True):
    t = [(i * 512, 512, False) for i in range(NL // 512)]
    if with_ctx:
        t.append((NL, NCX, True))
    return t


def build(stage=99, dbg=False):
    B = Builder(stage, dbg)
    nc, S = B.nc, B.S

    xT_d = B.dram_in("xT", [D, NT])
    cT_d = B.dram_in("cT", [D, 2])
    ada_w_d = B.dram_in("ada_w", [2, D, 6 * D])
    adab_d = B.dram_in("adab", [128, 2, 48])
    vecs_d = B.dram_in("vecs", [128, 64])
    cmat_d = B.dram_in("cmat", [128, 4, 128])
    masks_d = B.dram_in("masks", [128, 2, 128], I32)
    scanm_d = B.dram_in("scanm", [128, 512])
    segm_d = B.dram_in("segm", [128, 16])
    hw_in_d = B.dram_in("hgrn_w_in", [D, 5 * D])
    hw_out_d = B.dram_in("hgrn_w_out", [D, D])
    dw_in_d = B.dram_in("diff_w_in", [D, 3 * D])
    dw_out_d = B.dram_in("diff_w_out", [D, D])
    lvb_d = B.dram_in("lvb", [128, 256])
    rope_d = B.dram_in("rope", [128, 2, NL])
    cm2_d = B.dram_in("cm2", [128, 18, 128])
    router_d = B.dram_in("moe_router", [2, D, NE])
    wgate_d = B.dram_in("moe_w_gate", [2, NE, D, DEXP])
    wup_d = B.dram_in("moe_w_up", [2, NE, D, DEXP])
    wdown_d = B.dram_in("moe_w_down", [2, NE, DEXP, D])
    out_d = B.dram_out("outT", [D, NL])
    dbg_d = B.dram_out("dbg", [D, NT]) if dbg else None

    x_sb = B.sb([128, KD, NT], F32, "x")
    h_sb = B.sb([128, KD, NT], BF16, "h")
    cmat = B.sb([128, 4, 128], F32, "cmat")
    ident_f = cmat[:, 0, :]
    ident_b = B.sb([128, 128], BF16, "identb")
    ones_b = B.sb([128, 128], BF16, "onesb")
    mean_b = B.sb([128, 128], BF16, "meanb")
    m128_b = B.sb([128, 128], BF16, "m128b")
    masks = B.sb([128, 2, 128], I32, "masks")
    scanm = B.sb([128, 512], F32, "scanm")
    segm = B.sb([128, 16], F32, "segm")
    vecs = B.sb([128, 64], F32, "vecs")
    modT = B.sb([128, 2, 48, 2], F32, "modT")
    adab = B.sb([128, 2, 48], F32, "adab")
    coefA = B.sb([128, 4, 2, KD], F32, "coefA")
    siluc = B.sb([128, KD, 2], F32, "siluc")
    lb = B.sb([128, 8], F32, "lb")
    B.epsT = B.sb([128, 2], F32, "epsT")
    B.memset("pool", B.epsT[:], EPS, ["epsT"])
    oml = B.sb([128, 8], F32, "oml")

    PS = [nc.alloc_psum_tensor("ps%d" % i, [128, 512], F32) for i in range(8)]

    for k in range(KD):
        S.dma(x_sb[:, k, :], xT_d[k * 128:(k + 1) * 128, :], writes=["x%d" % k])
    S.dma(cmat[:], cmat_d, writes=["cmat"])
    S.dma(masks[:], masks_d, writes=["masks"])
    S.dma(scanm[:], scanm_d, writes=["scanm"])
    S.dma(segm[:], segm_d, writes=["segm"])
    S.dma(vecs[:], vecs_d, writes=["vecs"])
    S.dma(adab[:], adab_d, writes=["adab"])
    S.dma(siluc[:], cT_d.rearrange("(k p) c -> p k c", p=128), writes=["siluc"])
    B.cp("dve", ident_b[:], cmat[:, 0, :], ["cmat"], ["identb"])
    B.cp("dve", ones_b[:], cmat[:, 1, :], ["cmat"], ["onesb"])
    B.ts("dve", mean_b[:], cmat[:, 1, :], 1.0 / D, None, ALU.mult, None, ["cmat"], ["meanb"])
    B.ts("dve", m128_b[:], cmat[:, 1, :], 1.0 / 128, None, ALU.mult, None, ["cmat"], ["m128b"])
    B.act(siluc[:], siluc[:], AF.Silu, ["siluc"], ["siluc"])
    B.tt("dve", lb[:], vecs[:, 40:48], vecs[:, 48:56], ALU.subtract, ["vecs"], ["lb"])
    B.act(lb[:], lb[:], AF.Sigmoid, ["lb"], ["lb"])
    B.ts("dve", oml[:], lb[:], -1.0, 1.0, ALU.mult, ALU.add, ["lb"], ["oml"])

    m0 = B.mark()
    adaring = B.ring(2, [128, KD, 768], F32, "adaw")
    for i in range(2):
        for blk in range(8):
            wt, wk = adaring.next()
            S.dma(wt[:], ada_w_d[i, :, blk * 768:(blk + 1) * 768].rearrange("(k p) j -> p k j", p=128), writes=[wk])
            for jj in range(6):
                j = blk * 6 + jj
                for k in range(KD):
                    B.mm(PS[0][:, j * 2:(j + 1) * 2], wt[:, k, jj * 128:(jj + 1) * 128], siluc[:, k, :],
                         k == 0, k == KD - 1, [wk, "siluc"], ["ps0"])
        for col in range(2):
            B.tt("dve", modT[:, i, :, col], PS[0][:, 0:96].rearrange("p (j c) -> p j c", c=2)[:, :, col],
                 adab[:, i, :], ALU.add, ["ps0", "adab"], ["modT"])
    for n in range(4):
        layer, ffn = n // 2, n % 2
        gcol = (16 if ffn else 0) + 8 * layer
        sc0 = 32 if ffn else 8
        for col in range(2):
            B.stt("dve", coefA[:, n, col, :], modT[:, layer, sc0:sc0 + 8, col], 1.0, vecs[:, gcol:gcol + 8],
                  ALU.add, ALU.mult, ["modT", "vecs"], ["coefA"])
    B.release(m0)

    def norm_mod(n, with_ctx=True):
        layer, ffn = n // 2, n % 2
        sh0 = 24 if ffn else 0
        mk = B.mark()
        sqr = B.ring(3, [128, 512], BF16, "sq")
        rsr = B.ring(2, [128, 512], F32, "rstd")
        tmr = B.ring(3, [128, 512], F32, "ntmp")
        for (c0, cw, isctx) in col_tiles(with_ctx):
            col = 1 if isctx else 0
            for k in range(KD):
                sq, sqk = sqr.next()
                B.act(sq[:, :cw], x_sb[:, k, c0:c0 + cw], AF.Square, ["x%d" % k], [sqk])
                B.mm(PS[1][:, :cw], mean_b[:], sq[:, :cw], k == 0, k == KD - 1, [sqk, "meanb"], ["ps1"])
            rs, rsk = rsr.next()
            B.rsqrt(rs[:, :cw], PS[1][:, :cw], ["ps1"], rsk)
            for k in range(KD):
                tm, tmk = tmr.next()
                B.tt("dve", tm[:, :cw], x_sb[:, k, c0:c0 + cw], rs[:, :cw], ALU.mult, ["x%d" % k, rsk], [tmk])
                B.act(h_sb[:, k, c0:c0 + cw], tm[:, :cw], AF.Identity, [tmk, "coefA", "modT"], ["h%d" % k],
                      scale=coefA[:, n, col, k:k + 1], bias=modT[:, layer, sh0 + k, col:col + 1])
        B.release(mk)

    norm_mod(0)

    def finish(dump=None):
        if dbg and dump is not None:
            if dump == "x":
                for k in range(KD):
                    S.dma(dbg_d[k * 128:(k + 1) * 128, :], x_sb[:, k, :], reads=["x%d" % k], writes=["dbg%d" % k])
            else:
                mk = B.mark()
                hf = B.sb([128, NT], F32, "hf")
                for k in range(KD):
                    B.cp("dve", hf[:], h_sb[:, k, :], ["h%d" % k], ["hf"])
                    S.dma(dbg_d[k * 128:(k + 1) * 128, :], hf[:], reads=["hf"], writes=["dbg%d" % k])
            S.wait_all("sp", ["dbg%d" % k for k in range(KD)])
        for k in range(KD):
            S.dma(out_d[k * 128:(k + 1) * 128, :], x_sb[:, k, 0:NL], reads=["x%d" % k], writes=["out%d" % k])
        S.wait_all("sp", ["out%d" % k for k in range(KD)])
        S.emit()
        return nc

    if stage == 1:
        return finish("h")

    noml = B.sb([128, 8], F32, "noml")
    B.ts("dve", noml[:], oml[:], -1.0, None, ALU.mult, None, ["oml"], ["noml"])
    cin_h = [B.dram_tmp("cin_h%d" % i, [512, 256]) for i in range(4)]
    cout_h = [B.dram_tmp("cout_h%d" % i, [2048, 256]) for i in range(4)]
    PSB = nc.alloc_psum_tensor("psb", [128, 1024], BF16) if False else None
    PS4b = PS[4][:].bitcast(BF16)

    mh = B.mark()
    wst = B.ring(2, [128, KD, 128], F32, "wst")
    wbf = B.ring(6, [128, KD, 128], BF16, "wbf")
    q_sb = B.sb([128, NT], BF16, "q")
    g_sb = B.sb([128, NT], BF16, "g")
    v_tok = B.sb([128, NT // 128, 128], BF16, "vtok")
    o_acc = B.sb([128, NT], F32, "oacc")
    tmp = B.ring(6, [128, 512], F32, "tmp")
    qbr = B.ring(2, [128, 512], BF16, "qb")
    kbr = B.ring(2, [128, 512], BF16, "kb")
    kdr = B.ring(2, [128, 512], BF16, "kd")
    kdTr = B.ring(2, [128, 2, 4, 128], BF16, "kdT")
    for t_ in kdTr.aps:
        _, k_ = kdTr.next()
        B.memset("pool", t_[:], 0.0, [k_])
    sqr_ = B.ring(2, [128, 8, 128], BF16, "Sq")
    usbr = B.ring(2, [128, 2, 4, 128], F32, "Usb")
    aTf = B.ring(4, [128, 128], BF16, "aTf")
    aTb = B.ring(4, [128, 128], BF16, "aTb")
    for r_ in (aTf, aTb):
        for t_ in r_.aps:
            _, k_ = r_.next()
            B.memset("pool", t_[:], 0.0, [k_])
    srun = B.ring(3, [128, 128], F32, "srun")
    small = B.ring(8, [128, 8], F32, "small")
    lbuf = B.ring(1, [128, 4, 129], F32, "lbuf")
    onr = B.ring(2, [128, 512], BF16, "on")
    wo_st = B.sb([128, D], F32, "wost")
    wo_bf = B.sb([128, D], BF16, "wobf")
    totacc = B.sb([128, 8], F32, "totacc")

    def load_w(j):
        st_, stk = wst.next()
        S.dma(st_[:], hw_in_d[:, j * 128:(j + 1) * 128].rearrange("(k p) c -> p k c", p=128), writes=[stk])
        wb, wbk = wbf.next()
        B.cp("pool", wb[:], st_[:], [stk], [wbk])
        return wb, wbk

    def proj_fm(ps, psk, wb, wbk, c0, cw):
        for k in range(KD):
            B.mm(ps[:, :cw], wb[:, k, :], h_sb[:, k, c0:c0 + cw], k == 0, k == KD - 1, [wbk, "h%d" % k], [psk])

    import os
    HSTOP = int(os.environ.get("HSTOP", "99"))
    HFULL = int(os.environ.get("HFULL", "99"))

    def hgrn_dir(hd, d_, full, wf, wfk):
        lat_tiles = [(i * 512, 512, False) for i in range(4)]
        if d_ == 1:
            lat_tiles = lat_tiles[::-1]
        tiles = ([(NL, NCX, True)] if full else []) + lat_tiles
        S_cur, S_k = srun.next()
        B.memset("pool", S_cur[:], 0.0, [S_k])
        aTr = aTf if d_ == 0 else aTb
        first_lat = True
        ti = 0
        for (c0, cw, isctx) in tiles:
            nch = cw // 64
            nsub = cw // 128
            if full and (not isctx) and first_lat:
                lb_, lbk = lbuf.next()
                for s_ in range(4):
                    p_ = hd * 2 + d_
                    r0 = s_ * 512 + (p_ % 4) * 128
                    S.dma(lb_[:, s_, :], cout_h[p_ // 4][r0:r0 + 128, 0:129], reads=["cout_h%d" % (p_ // 4)], writes=[lbk])
                order = [0, 1, 2, 3] if d_ == 0 else [3, 2, 1, 0]
                for s_ in order:
                    mcol = (0 if d_ == 0 else 4) + s_
                    sm, smk = small.next()
                    B.ts("dve", sm[:, 0:1], lb_[:, s_, 128:129], segm[:, mcol:mcol + 1], segm[:, 8 + mcol:9 + mcol],
                         ALU.mult, ALU.add, [lbk, "segm"], [smk])
                    t1, t1k = tmp.next()
                    B.ts("dve", t1[:, :128], lb_[:, s_, 0:128], segm[:, mcol:mcol + 1], None, ALU.mult, None,
                         [lbk, "segm"], [t1k])
                    S_n, S_nk = srun.next()
                    B.stt("dve", S_n[:], S_cur[:], sm[:, 0:1], t1[:, :128], ALU.mult, ALU.add, [S_k, smk, t1k], [S_nk])
                    S_cur, S_k = S_n, S_nk
            if not isctx:
                first_lat = False
            psF, psFk = (PS[1], "ps1") if d_ == 0 else (PS[2], "ps2")
            proj_fm(psF, psFk, wf, wfk, c0, cw)
            t_sig, k_sig = tmp.next()
            B.act(t_sig[:, :cw], psF[:, :cw], AF.Sigmoid, [psFk], [k_sig])
            t_lf, k_lf = tmp.next()
            B.act(t_lf[:, :cw], t_sig[:, :cw], AF.Ln, [k_sig, "oml", "lb"], [k_lf],
                  scale=oml[:, hd:hd + 1], bias=lb[:, hd:hd + 1])
            t_kk, k_kk = tmp.next()
            B.ts("dve", t_kk[:, :cw], t_sig[:, :cw], noml[:, hd:hd + 1], oml[:, hd:hd + 1], ALU.mult, ALU.add,
                 [k_sig, "noml", "oml"], [k_kk])
            t_b, k_b = tmp.next()
            S.op("dve", lambda e, o=t_b[:, :cw], a=scanm[:, :cw], b_=t_lf[:, :cw]:
                 e.tensor_tensor_scan(out=o, data0=a, data1=b_, initial=0.0, op0=ALU.mult, op1=ALU.add),
                 reads=["scanm", k_lf], writes=[k_b])
            b3 = t_b[:, :cw].rearrange("p (c s) -> p c s", s=64)
            tot, totk = small.next()
            B.cp("dve", tot[:, :nch], b3[:, :, 63], [k_b], [totk])
            tot_bc = tot[:, :nch].unsqueeze(2).broadcast_to([128, nch, 64])
            if d_ == 1:
                t_c, k_c = tmp.next()
                c3 = t_c[:, :cw].rearrange("p (c s) -> p c s", s=64)
                B.tt("dve", c3, tot_bc, b3, ALU.subtract, [totk, k_b], [k_c])
                B.tt("dve", t_c[:, :cw], t_c[:, :cw], t_lf[:, :cw], ALU.add, [k_c, k_lf], [k_c])
            else:
                t_c, k_c, c3 = t_b, k_b, b3
            t_d3, k_d3 = tmp.next()
            d33 = t_d3[:, :cw].rearrange("p (c s) -> p c s", s=64)
            B.tt("dve", d33, tot_bc, c3, ALU.subtract, [totk, k_c], [k_d3])
            B.act(t_d3[:, :cw], t_d3[:, :cw], AF.Exp, [k_d3], [k_d3])
            kd, kdk = kdr.next()
            B.tt("pool", kd[:, :cw], t_kk[:, :cw], t_d3[:, :cw], ALU.mult, [k_kk, k_d3], [kdk])
            Dc, Dck = small.next()
            B.act(Dc[:, :nch], tot[:, :nch], AF.Exp, [totk], [Dck])
            if not full:
                B.S.op("dve", lambda e, o=totacc[:, ti:ti + 1], i_=tot[:, :nch]:
                       e.tensor_reduce(out=o, in_=i_, axis=AX.X, op=ALU.add), reads=[totk], writes=["totacc"])
            if full:
                cmid_bc = c3[:, :, 32:33].broadcast_to([128, nch, 64])
                t_d1, k_d1 = tmp.next()
                d13 = t_d1[:, :cw].rearrange("p (c s) -> p c s", s=64)
                B.tt("dve", d13, c3, cmid_bc, ALU.subtract, [k_c], [k_d1])
                t_e1, k_e1 = tmp.next()
                B.act(t_e1[:, :cw], t_d1[:, :cw], AF.Exp, [k_d1], [k_e1])
                qb, qbk = qbr.next()
                B.tt("pool", qb[:, :cw], q_sb[:, c0:c0 + cw], t_e1[:, :cw], ALU.mult, ["q", k_e1], [qbk])
                t_e2, k_e2 = t_d1, k_d1
                B.act(t_e2[:, :cw], t_d1[:, :cw], AF.Exp, [k_d1], [k_e2], scale=-1.0)
                kb, kbk = kbr.next()
                B.tt("pool", kb[:, :cw], t_kk[:, :cw], t_e2[:, :cw], ALU.mult, [k_kk, k_e2], [kbk])
                ebr, ebrk = small.next()
                B.act(ebr[:, :nch], c3[:, :, 32], AF.Exp, [k_c], [ebrk])
            if HSTOP <= 1:
                continue
            kdT, kdTk = kdTr.next()
            for sj in range(nsub):
                B.tr(PS4b[:, sj * 128:(sj + 1) * 128], kd[:, sj * 128:(sj + 1) * 128], ident_b[:], [kdk, "identb"], ["ps4"])
            for half in range(2):
                pr = slice(half * 64, (half + 1) * 64)
                B.cp("dve", kdT[pr, half, :nsub, :], PS4b[pr, :nsub * 128].rearrange("p (j k) -> p j k", k=128),
                     ["ps4"], [kdTk])
            if HSTOP <= 2:
                continue
            tj0 = c0 // 128
            ubank = []
            for ch in range(nch):
                sj, half = ch // 2, ch % 2
                UB = [int(v) for v in os.environ.get("UB", "5,6").split(",")]
                bank, bk = (PS[UB[0]], "ps%d" % UB[0]) if half == 0 else (PS[UB[1]], "ps%d" % UB[1])
                B.mm(bank[:, sj * 128:(sj + 1) * 128], kdT[:, half, sj, :],
                     v_tok[:, tj0 + sj, :], True, True, [kdTk, "vtok"], [bk + "_%d" % sj])
                ubank.append((bank[:, sj * 128:(sj + 1) * 128], bk + "_%d" % sj))
            if HSTOP <= 3:
                continue
            if True:
                Usb, Usbk = usbr.next()
                UB = [int(v) for v in os.environ.get("UB", "5,6").split(",")]
                for half in range(2):
                    B.cp("dve" if half == 0 else "act", Usb[:, half, :nsub, :],
                         PS[UB[half]][:, :nsub * 128].rearrange("p (j k) -> p j k", k=128),
                         ["ps%d_%d" % (UB[half], j_) for j_ in range(nsub)], [Usbk + "_%d" % half])
                ubank = [(Usb[:, ch % 2, ch // 2, :], Usbk + "_%d" % (ch % 2)) for ch in range(nch)]
            Sq, Sqk = sqr_.next()
            chs = list(range(nch)) if d_ == 0 else list(range(nch))[::-1]
            CHN = int(os.environ.get("CHN", "99"))
            CHMODE = int(os.environ.get("CHMODE", "3"))
            for ch in chs[:CHN]:
                if full:
                    B.ts("pool", Sq[:, ch, :], S_cur[:], ebr[:, ch:ch + 1], None, ALU.mult, None, [S_k, ebrk], [Sqk + "_%d" % ch])
                S_n, S_nk = srun.next()
                if CHMODE & 1:
                    B.ts("dve", S_n[:], S_cur[:], Dc[:, ch:ch + 1], None, ALU.mult, None, [S_k, Dck], [S_nk])
                if CHMODE & 2:
                    B.tt("dve", S_n[:], ubank[ch][0], S_n[:], ALU.add, [ubank[ch][1], S_nk], [S_nk])
                if CHMODE & 4:
                    B.cp("act", S_n[:], ubank[ch][0], [ubank[ch][1]], [S_nk])
                if CHMODE & 8:
                    B.cp("dve", S_n[:], ubank[ch][0], [ubank[ch][1]], [S_nk])
                S_cur, S_k = S_n, S_nk
            if full and HFULL >= 1:
                mask_ap = masks[:, d_, :]
                for sj in range(nsub):
                    cs = slice(sj * 128, (sj + 1) * 128)
                    B.mm(PS[3][:, cs], kb[:, cs], qb[:, cs], True, True, [kbk, qbk], ["ps3"])
                aTs = []
                for sj in range(nsub):
                    cs = slice(sj * 128, (sj + 1) * 128)
                    aT, aTk = aTr.next()
                    S.op("dve", lambda e, o=aT[:], m_=mask_ap, d__=PS[3][:, cs]: e.copy_predicated(out=o, mask=m_, data=d__),
                         reads=["ps3", "masks"], writes=[aTk])
                    aTs.append((aT, aTk))
                for sj in range(nsub):
                    cs = slice(sj * 128, (sj + 1) * 128)
                    aT, aTk = aTs[sj]
                    B.mm(PS[7][:, cs], v_tok[:, tj0 + sj, :], aT[:], True, False, ["vtok", aTk], ["ps7"])
                    for half in range(2):
                        ch = sj * 2 + half
                        cc = slice(ch * 64, (ch + 1) * 64)
                        B.mm(PS[7][:, cc], Sq[:, ch, :], qb[:, cc], False, half == 1, [Sqk + "_%d" % ch, qbk], ["ps7"])
                if d_ == 0:
                    B.cp("act", o_acc[:, c0:c0 + cw], PS[7][:, :cw], ["ps7"], ["oacc%d" % (c0 // 512)])
                else:
                    B.tt("dve", o_acc[:, c0:c0 + cw], o_acc[:, c0:c0 + cw], PS[7][:, :cw], ALU.add,
                         ["ps7", "oacc%d" % (c0 // 512)], ["oacc%d" % (c0 // 512)])
            ti += 1
        return S_cur, S_k

    for hd in range({14: 0, 16: 1}.get(stage, 8)):
        wv, wvk = load_w(24 + hd)
        wfs = [load_w(8 + hd), load_w(16 + hd)]
        for tj in range(NL // 128):
            bank_col = (tj % 4) * 128
            for k in range(KD):
                B.mm(PS[3][:, bank_col:bank_col + 128], h_sb[:, k, tj * 128:(tj + 1) * 128], wv[:, k, :],
                     k == 0, k == KD - 1, ["h%d" % k, wvk], ["ps3"])
            if tj % 4 == 3:
                B.cp("act", v_tok[:, tj - 3:tj + 1, :], PS[3][:].rearrange("p (j v) -> p j v", v=128), ["ps3"], ["vtok"])
        for d_ in range(2):
            S_f, S_fk = hgrn_dir(hd, d_, False, wfs[d_][0], wfs[d_][1])
            if HSTOP <= 4:
                continue
            p_ = hd * 2 + d_
            ci, r0 = p_ // 4, (p_ % 4) * 128
            S.dma(cin_h[ci][r0:r0 + 128, 0:128], S_f[:], reads=[S_fk], writes=["cin_h%d" % ci], slot="cin_hs%d" % (d_))
            sm, smk = small.next()
            S.op("dve", lambda e, o=sm[:, 0:1], i_=totacc[:, 0:4]: e.tensor_reduce(out=o, in_=i_, axis=AX.X, op=ALU.add),
                 reads=["totacc"], writes=[smk])
            B.act(sm[:, 1:2], sm[:, 0:1], AF.Exp, [smk], [smk])
            S.dma(cin_h[ci][r0:r0 + 128, 128:129], sm[:, 1:2], reads=[smk], writes=["cin_h%d" % ci], slot="cin_hd%d" % (d_),
                  allow_slow_non_contiguous=True)
    import os
    if not os.environ.get("NOCC"):
        for ci in range(4):
            S.coll(lambda e, ci=ci: e.collective_compute("AllGather", ALU.bypass, replica_groups=[[0, 1, 2, 3], [4, 5, 6, 7]],
                                                        ins=[cin_h[ci]], outs=[cout_h[ci]]),
                 reads=["cin_h%d" % ci], writes=["cout_h%d" % ci])

    if not os.environ.get("NOCC"):
        cin_zh = B.dram_tmp("cin_zh", [16, 64])
        cout_zh = B.dram_tmp("cout_zh", [64, 64])
        S.dma(cin_zh, segm[0:16, 0:16].bitcast(F32) if False else vecs[0:16, 0:64], reads=["vecs"] + ["cout_h%d" % i for i in range(4)], writes=["cin_zh"])
        S.coll(lambda e: e.collective_compute("AllGather", ALU.bypass, replica_groups=[[0, 1, 2, 3], [4, 5, 6, 7]],
                                              ins=[cin_zh], outs=[cout_zh]),
               reads=["cin_zh"] + ["cout_h%d" % i for i in range(4)], writes=["cout_zh"] + ["cout_h%d" % i for i in range(4)])
    if stage in (14, 15, 16):
        return finish("x")
    for hd in range(8):
        wq, wqk = load_w(hd)
        wg, wgk = load_w(32 + hd)
        wv, wvk = load_w(24 + hd)
        wfs = [load_w(8 + hd), load_w(16 + hd)]
        S.dma(wo_st[:], hw_out_d[hd * 128:(hd + 1) * 128, :], writes=["wost"])
        B.cp("pool", wo_bf[:], wo_st[:], ["wost"], ["wobf"])
        for (c0, cw, isctx) in col_tiles():
            proj_fm(PS[0], "ps0", wq, wqk, c0, cw)
            B.act(q_sb[:, c0:c0 + cw], PS[0][:, :cw], AF.Silu, ["ps0"], ["q"])
            proj_fm(PS[0], "ps0", wg, wgk, c0, cw)
            B.act(g_sb[:, c0:c0 + cw], PS[0][:, :cw], AF.Silu, ["ps0"], ["g"])
        for tj in range(NT // 128):
            bank_col = (tj % 4) * 128
            for k in range(KD):
                B.mm(PS[3][:, bank_col:bank_col + 128], h_sb[:, k, tj * 128:(tj + 1) * 128], wv[:, k, :],
                     k == 0, k == KD - 1, ["h%d" % k, wvk], ["ps3"])
            if tj % 4 == 3 or tj == NT // 128 - 1:
                n_ = tj % 4 + 1
                B.cp("act", v_tok[:, tj - n_ + 1:tj + 1, :], PS[3][:, :n_ * 128].rearrange("p (j v) -> p j v", v=128),
                     ["ps3"], ["vtok"])
        for d_ in range(2):
            hgrn_dir(hd, d_, True, wfs[d_][0], wfs[d_][1])
        for (c0, cw, isctx) in (col_tiles() if HFULL >= 2 else []):
            col = 1 if isctx else 0
            ok = "oacc%d" % (c0 // 512)
            sq, sqk = qbr.next()
            B.act(sq[:, :cw], o_acc[:, c0:c0 + cw], AF.Square, [ok], [sqk])
            B.mm(PS[1][:, :cw], m128_b[:], sq[:, :cw], True, True, [sqk, "m128b"], ["ps1"])
            rs, rsk = tmp.next()
            B.rsqrt(rs[:, :cw], PS[1][:, :cw], ["ps1"], rsk)
            t1, t1k = tmp.next()
            B.tt("dve", t1[:, :cw], o_acc[:, c0:c0 + cw], rs[:, :cw], ALU.mult, [ok, rsk], [t1k])
            on, onk = onr.next()
            B.stt("dve", on[:, :cw], t1[:, :cw], vecs[:, 56:57], g_sb[:, c0:c0 + cw], ALU.mult, ALU.mult,
                  [t1k, "vecs", "g"], [onk])
            for dk in range(KD):
                bank, bk = (PS[0], "ps0") if dk % 2 == 0 else (PS[2], "ps2")
                B.mm(bank[:, :cw], wo_bf[:, dk * 128:(dk + 1) * 128], on[:, :cw], True, True, ["wobf", onk], [bk])
                B.stt("dve", x_sb[:, dk, c0:c0 + cw], bank[:, :cw], modT[:, 0, 16 + dk, col:col + 1],
                      x_sb[:, dk, c0:c0 + cw], ALU.mult, ALU.add, [bk, "modT", "x%d" % dk], ["x%d" % dk])
    B.release(mh)

    if stage == 2:
        return finish("x")

    def moe_layer(L, with_ctx):
        n = 2 * L + 1
        norm_mod(n, with_ctx)
        mk = B.mark()
        cm2 = B.sb([128, 18, 128], F32, "cm2")
        S.dma(cm2[:], cm2_d, writes=["cm2"])
        tiles = col_tiles(with_ctx)
        ncols = NT if with_ctx else NL
        gw = B.sb([128, NT], F32, "gw")
        mk2 = B.mark()
        wr_st = B.sb([128, KD, NE], F32, "wrst")
        wr_bf = B.sb([128, KD, 128], BF16, "wrbf")
        S.dma(wr_st[:], router_d[L].rearrange("(k p) e -> p k e", p=128), writes=["wrst"])
        B.memset("pool", wr_bf[:], 0.0, ["wrbf"])
        B.cp("pool", wr_bf[:, :, 0:NE], wr_st[:], ["wrst", "wrbf"], ["wrbf"])
        etile = B.ring(2, [128, 512], F32, "etile")
        for (c0, cw, isctx) in tiles:
            for k in range(KD):
                B.mm(PS[0][:, :cw], wr_bf[:, k, :], h_sb[:, k, c0:c0 + cw], k == 0, k == KD - 1, ["wrbf", "h%d" % k], ["ps0"])
            et, etk = etile.next()
            B.act(et[:, :cw], PS[0][:, :cw], AF.Exp, ["ps0"], [etk])
            B.mm(PS[1][:, :cw], cm2[:, 16, :], et[:, :cw], True, True, ["cm2", etk], ["ps1"])
            rt, rtk = etile.next()
            S.op("dve", lambda e, o=rt[:, :cw], i_=PS[1][:, :cw]: e.reciprocal(out=o, in_=i_), reads=["ps1"], writes=[rtk])
            B.tt("dve", gw[:, c0:c0 + cw], et[:, :cw], rt[:, :cw], ALU.mult, [etk, rtk], ["gw"])
        cin_a = B.dram_tmp("cin_a%d" % L, [NE, NL])
        cout_a = B.dram_tmp("cout_a%d" % L, [4 * NE, NL])
        S.dma(cin_a, gw[0:NE, 0:NL], reads=["gw"], writes=["cin_a"])
        S.coll(lambda e: e.collective_compute("AllGather", ALU.bypass, replica_groups=[[0, 1, 2, 3], [4, 5, 6, 7]],
                                              ins=[cin_a], outs=[cout_a]), reads=["cin_a"], writes=["cout_a"])
        cin_z = B.dram_tmp("cin_z%d" % L, [NE, 64])
        cout_z = B.dram_tmp("cout_z%d" % L, [4 * NE, 64])
        S.dma(cin_z, gw[0:NE, 0:64], reads=["gw", "cout_a"], writes=["cin_z"])
        S.coll(lambda e: e.collective_compute("AllGather", ALU.bypass, replica_groups=[[0, 1, 2, 3], [4, 5, 6, 7]],
                                              ins=[cin_z], outs=[cout_z]), reads=["cin_z", "cout_a"], writes=["cout_z", "cout_a"])
        A = B.sb([128, NL], F32, "A")
        B.memset("pool", A[:], 0.0, ["A"])
        S.dma(A[0:64, :], cout_a, reads=["cout_a", "A"], writes=["A"])
        if with_ctx:
            S.dma(A[64:80, 0:NCX], gw[0:NE, NL:NT], reads=["gw", "A"], writes=["A"])
        msk = B.sb([128, NL], F32, "msk")
        thr = B.sb([128, 1], F32, "thr")
        B.memset("pool", thr[:], 0.0, ["thr"])
        sm_ = B.ring(4, [128, 2], F32, "thsm")
        sm8 = B.ring(2, [128, 8], F32, "thsm8")
        for it in range(1, 31):
            step = 2.0 ** (-it)
            cand, ck = sm_.next()
            B.ts("dve", cand[:, 0:1], thr[:], step, None, ALU.add, None, ["thr"], [ck])
            B.ts("dve", msk[:], A[:], cand[:, 0:1], None, ALU.is_ge, None, ["A", ck], ["msk"])
            c8, c8k = sm8.next()
            S.op("dve", lambda e, o=c8[:]: e.tensor_reduce(out=o, in_=msk[:].rearrange("p (g c) -> p g c", c=256),
                                                          axis=AX.X, op=ALU.add), reads=["msk"], writes=[c8k])
            B.mm(PS[2][:, 0:8], cm2[:, 17, :], c8[:], True, True, ["cm2", c8k], ["ps2"])
            S.op("dve", lambda e, o=cand[:, 1:2]: e.tensor_reduce(out=o, in_=PS[2][:, 0:8], axis=AX.X, op=ALU.add),
                 reads=["ps2"], writes=[ck])
            ge, gk = sm_.next()
            B.tt("dve", ge[:, 0:1], cand[:, 1:2], vecs[:, 58:59], ALU.is_ge, [ck, "vecs"], [gk])
            B.stt("dve", thr[:], ge[:, 0:1], step, thr[:], ALU.mult, ALU.add, [gk, "thr"], ["thr"])
        B.stt("dve", gw[0:NE, 0:NL], gw[0:NE, 0:NL], thr[0:NE, 0:1], gw[0:NE, 0:NL], ALU.is_ge, ALU.mult, ["gw", "thr"], ["gw"])
        if with_ctx:
            thc = B.sb([128, 1], F32, "thc")
            S.dma(thc[0:NE, 0:1], thr[64:80, 0:1], reads=["thr"], writes=["thc"])
            B.stt("dve", gw[0:NE, NL:NT], gw[0:NE, NL:NT], thc[0:NE, 0:1], gw[0:NE, NL:NT], ALU.is_ge, ALU.mult,
                  ["gw", "thc"], ["gw"])
        B.ts("pool", gw[:], gw[:], cm2[:, 16, 0:1], None, ALU.mult, None, ["gw", "cm2"], ["gw"])
        B.release(mk2)
        gwbr = B.ring(2, [128, NT], F32, "gwb")
        wgs = B.ring(2, [128, KD, 128], F32, "wgs")
        wus = B.ring(2, [128, KD, 128], F32, "wus")
        wds = B.ring(2, [128, D], F32, "wds")
        wgb = B.ring(2, [128, KD, 128], BF16, "wgb")
        wub = B.ring(2, [128, KD, 128], BF16, "wub")
        wdb = B.ring(2, [128, D], BF16, "wdb")
        sar = B.ring(2, [128, 512], F32, "sa")
        actr = B.ring(2, [128, 512], BF16, "actb")
        ysr = B.ring(3, [128, 512], F32, "ysb")
        psa = Ring([PS[0], PS[1]], "psA")
        psu = Ring([PS[2], PS[3]], "psU")
        psy = Ring([PS[4], PS[5], PS[6], PS[7]], "psY")
        NACT = 2
        NEXP = int(os.environ.get("NEXP", str(NE)))
        items = [(e_, fb) for e_ in range(NEXP) for fb in range(NF)]

        def load_item(i):
            e_, fb = items[i]
            g_s, g_sk = wgs.next(); u_s, u_sk = wus.next(); d_s, d_sk = wds.next()
            S.dma(g_s[:], wgate_d[L, e_, :, fb * 128:(fb + 1) * 128].rearrange("(k p) f -> p k f", p=128), writes=[g_sk])
            S.dma(u_s[:], wup_d[L, e_, :, fb * 128:(fb + 1) * 128].rearrange("(k p) f -> p k f", p=128), writes=[u_sk])
            S.dma(d_s[:], wdown_d[L, e_, fb * 128:(fb + 1) * 128, :], writes=[d_sk])
            g_b, g_bk = wgb.next(); u_b, u_bk = wub.next(); d_b, d_bk = wdb.next()
            B.cp("pool", g_b[:], g_s[:], [g_sk], [g_bk])
            B.cp("pool", u_b[:], u_s[:], [u_sk], [u_bk])
            B.cp("act", d_b[:], d_s[:], [d_sk], [d_bk])
            return (g_b, g_bk, u_b, u_bk, d_b, d_bk)

        units = [(i, t) for i in range(len(items)) for t in range(len(tiles))]
        loaded = {0: load_item(0)}
        if len(items) > 1:
            loaded[1] = load_item(1)
        next_to_load = 2
        gwb_of = {}

        def emit_proj(u):
            i, t = units[u]
            e_, fb = items[i]
            c0, cw, isctx = tiles[t]
            if fb == 0 and t == 0:
                gwb, gwbk = gwbr.next()
                for (c0_, cw_, _x) in tiles:
                    pa, pak = psa.next()
                    B.mm(pa[:, :cw_], cm2[:, e_, :], gw[:, c0_:c0_ + cw_], True, True, ["cm2", "gw"], [pak])
                    B.cp("act", gwb[:, c0_:c0_ + cw_], pa[:, :cw_], [pak], [gwbk])
                gwb_of[e_] = (gwb, gwbk)
            gwb, gwbk = gwb_of[e_]
            g_b, g_bk, u_b, u_bk, d_b, d_bk = loaded[i]
            pa, pak = psa.next(); pu, puk = psu.next()
            for k in range(KD):
                B.mm(pa[:, :cw], g_b[:, k, :], h_sb[:, k, c0:c0 + cw], k == 0, k == KD - 1, [g_bk, "h%d" % k], [pak])
            for k in range(KD):
                B.mm(pu[:, :cw], u_b[:, k, :], h_sb[:, k, c0:c0 + cw], k == 0, k == KD - 1, [u_bk, "h%d" % k], [puk])
            sa, sak = sar.next()
            B.act(sa[:, :cw], pa[:, :cw], AF.Silu, [pak], [sak])
            B.tt("dve", sa[:, :cw], sa[:, :cw], pu[:, :cw], ALU.mult, [sak, puk], [sak])
            ab, abk = actr.next()
            B.tt("pool", ab[:, :cw], sa[:, :cw], gwb[:, c0:c0 + cw], ALU.mult, [sak, gwbk], [abk])
            return ab, abk

        def emit_down(u, ab, abk):
            i, t = units[u]
            c0, cw, isctx = tiles[t]
            col = 1 if isctx else 0
            g_b, g_bk, u_b, u_bk, d_b, d_bk = loaded[i]
            for dk in range(KD):
                py, pyk = psy.next()
                B.mm(py[:, :cw], d_b[:, dk * 128:(dk + 1) * 128], ab[:, :cw], True, True, [d_bk, abk], [pyk])
                gsc = modT[:, L, 40 + dk, col:col + 1]
                if dk < NACT:
                    ys, ysk = ysr.next()
                    B.act(ys[:, :cw], py[:, :cw], AF.Identity, [pyk, "modT"], [ysk], scale=gsc)
                    B.tt("pool", x_sb[:, dk, c0:c0 + cw], x_sb[:, dk, c0:c0 + cw], ys[:, :cw], ALU.add,
                         ["x%d" % dk, ysk], ["x%d" % dk])
                else:
                    B.stt("dve", x_sb[:, dk, c0:c0 + cw], py[:, :cw], gsc, x_sb[:, dk, c0:c0 + cw],
                          ALU.mult, ALU.add, [pyk, "modT", "x%d" % dk], ["x%d" % dk])

        cur = emit_proj(0)
        for u in range(len(units)):
            nxt = emit_proj(u + 1) if u + 1 < len(units) else None
            emit_down(u, cur[0], cur[1])
            i, t = units[u]
            if t == len(tiles) - 1:
                del loaded[i]
                if next_to_load < len(items):
                    loaded[next_to_load] = load_item(next_to_load)
                    next_to_load += 1
            cur = nxt
        B.release(mk)

    moe_layer(0, True)
    if stage == 3:
        return finish("x")

    LAM_INIT = 0.8 - 0.6 * math.exp(-0.3 * 1)
    norm_mod(2, True)
    ma = B.mark()
    q_all = B.sb([128, 8, NL], BF16, "qall")
    kctx = B.sb([128, 8, NCX], BF16, "kctx")
    vctx = B.sb([128, 8, 2, 128], BF16, "vctx")
    nlam = B.sb([128, 4], F32, "nlam")
    mlam = B.mark()
    lvb = B.sb([128, 256], F32, "lvb")
    S.dma(lvb[:], lvb_d, writes=["lvb"])
    lvp = B.sb([128, 128], F32, "lvp")
    B.tt("dve", lvp[:, 0:64], lvb[:, 0:64], lvb[:, 64:128], ALU.mult, ["lvb"], ["lvp"])
    B.tt("dve", lvp[:, 64:128], lvb[:, 128:192], lvb[:, 192:256], ALU.mult, ["lvb"], ["lvp"])
    S.op("dve", lambda e: e.tensor_reduce(out=nlam[:, 2:3], in_=lvp[:, 0:64], axis=AX.X, op=ALU.add), reads=["lvp"], writes=["nlam"])
    S.op("dve", lambda e: e.tensor_reduce(out=nlam[:, 3:4], in_=lvp[:, 64:128], axis=AX.X, op=ALU.add), reads=["lvp"], writes=["nlam"])
    B.act(nlam[:, 2:4], nlam[:, 2:4], AF.Exp, ["nlam"], ["nlam"])
    B.tt("dve", nlam[:, 0:1], nlam[:, 3:4], nlam[:, 2:3], ALU.subtract, ["nlam"], ["nlam"])
    B.ts("dve", nlam[:, 0:1], nlam[:, 0:1], -LAM_INIT, None, ALU.add, None, ["nlam"], ["nlam"])
    B.ts("dve", nlam[:, 1:2], vecs[:, 57:58], 1.0 - LAM_INIT, None, ALU.mult, None, ["vecs"], ["nlam"])
    B.release(mlam)

    cin_k = [B.dram_tmp("cin_k%d" % i, [128, NL], BF16) for i in range(8)]
    cout_k = [B.dram_tmp("cout_k%d" % i, [512, NL], BF16) for i in range(8)]
    cin_v = [B.dram_tmp("cin_v%d" % i, [NL, 128], BF16) for i in range(8)]
    cout_v = [B.dram_tmp("cout_v%d" % i, [4 * NL, 128], BF16) for i in range(8)]

    mpa = B.mark()
    rope = B.sb([128, 2, NL], F32, "rope")
    S.dma(rope[:], rope_d, writes=["rope"])
    wst2 = B.ring(2, [128, KD, 128], F32, "wst2")
    wbf2 = B.ring(3, [128, KD, 128], BF16, "wbf2")
    rtmp = B.ring(4, [128, 512], F32, "rtmp")
    kT_loc = B.ring(1, [128, NL], BF16, "kTloc")
    v_loc = B.ring(1, [128, NL // 128, 128], BF16, "vloc")

    def load_w2(j):
        st_, stk = wst2.next()
        S.dma(st_[:], dw_in_d[:, j * 128:(j + 1) * 128].rearrange("(k p) c -> p k c", p=128), writes=[stk])
        wb, wbk = wbf2.next()
        B.cp("pool", wb[:], st_[:], [stk], [wbk])
        return wb, wbk

    def rope_tile(ps, psk, c0, out_ap, outk):
        xf, xfk = rtmp.next()
        B.cp("act", xf[:], ps[:], [psk], [xfk])
        B.mm(PS[2][:], cmat[:, 2, :], xf[:], True, True, ["cmat", xfk], ["ps2"])
        t1, t1k = rtmp.next()
        B.tt("pool", t1[:], xf[:], rope[:, 0, c0:c0 + 512], ALU.mult, [xfk, "rope"], [t1k])
        t2, t2k = rtmp.next()
        B.tt("dve", t2[:], PS[2][:], rope[:, 1, c0:c0 + 512], ALU.mult, ["ps2", "rope"], [t2k])
        B.tt("dve", out_ap, t1[:], t2[:], ALU.add, [t1k, t2k], [outk])

    for hd in range(8):
        wq, wqk = load_w2(hd)
        wk_, wkk = load_w2(8 + hd)
        wv, wvk = load_w2(16 + hd)
        kT, kTk = kT_loc.next()
        for (c0, cw, isctx) in col_tiles(True):
            if not isctx:
                proj_fm(PS[0], "ps0", wq, wqk, c0, cw)
                rope_tile(PS[0], "ps0", c0, q_all[:, hd, c0:c0 + cw], "qall%d" % hd)
                proj_fm(PS[1], "ps1", wk_, wkk, c0, cw)
                rope_tile(PS[1], "ps1", c0, kT[:, c0:c0 + cw], kTk)
            else:
                proj_fm(PS[1], "ps1", wk_, wkk, c0, cw)
                B.cp("act", kctx[:, hd, :], PS[1][:, :cw], ["ps1"], ["kctx"])
        S.dma(cin_k[hd], kT[:], reads=[kTk], writes=["cin_k%d" % hd])
        S.coll(lambda e, hd=hd: e.collective_compute("AllGather", ALU.bypass, replica_groups=[[0, 1, 2, 3], [4, 5, 6, 7]],
                                                      ins=[cin_k[hd]], outs=[cout_k[hd]]),
             reads=["cin_k%d" % hd], writes=["cout_k%d" % hd])
        vl, vlk = v_loc.next()
        for tj in range(NT // 128):
            bank_col = (tj % 4) * 128
            for k in range(KD):
                B.mm(PS[3][:, bank_col:bank_col + 128], h_sb[:, k, tj * 128:(tj + 1) * 128], wv[:, k, :],
                     k == 0, k == KD - 1, ["h%d" % k, wvk], ["ps3"])
            if tj % 4 == 3 and tj < 16:
                B.cp("act", vl[:, tj - 3:tj + 1, :], PS[3][:].rearrange("p (j v) -> p j v", v=128), ["ps3"], [vlk])
            if tj == 17:
                B.cp("act", vctx[:, hd, :, :], PS[3][:, 0:256].rearrange("p (j v) -> p j v", v=128), ["ps3"], ["vctx"])
        S.dma(cin_v[hd].rearrange("(j p) v -> p j v", p=128), vl[:], reads=[vlk], writes=["cin_v%d" % hd])
        S.coll(lambda e, hd=hd: e.collective_compute("AllGather", ALU.bypass, replica_groups=[[0, 1, 2, 3], [4, 5, 6, 7]],
                                                      ins=[cin_v[hd]], outs=[cout_v[hd]]),
             reads=["cin_v%d" % hd], writes=["cout_v%d" % hd])
    cin_zk = B.dram_tmp("cin_zk", [16, 64])
    cout_zk = B.dram_tmp("cout_zk", [64, 64])
    allkv = ["cout_k%d" % i for i in range(8)] + ["cout_v%d" % i for i in range(8)]
    S.dma(cin_zk, vecs[0:16, 0:64], reads=["vecs"] + allkv, writes=["cin_zk"])
    S.coll(lambda e: e.collective_compute("AllGather", ALU.bypass, replica_groups=[[0, 1, 2, 3], [4, 5, 6, 7]],
                                          ins=[cin_zk], outs=[cout_zk]), reads=["cin_zk"] + allkv, writes=["cout_zk"] + allkv)
    B.release(mpa)
    if stage == 35:
        return finish("x")

    NKT = (4 * NL + NCX) // 128
    h_off = B.SB_BASE + 128 * 0 + KD * NT * 4
    K01 = nc.alloc_sbuf_tensor_at("K01", [128, 2, NKT * 128], BF16, offset=h_off)
    B.memset("pool", K01[64:128, 0, :], 0.0, ["K01"])
    B.memset("pool", K01[0:64, 1, :], 0.0, ["K01"])
    V_h = B.sb([128, NKT, 128], BF16, "Vh")
    er = B.ring(4, [128, 512], BF16, "etl")
    ftmp = B.ring(6, [128, 512], F32, "ftmp")
    onr2 = B.ring(2, [128, 512], BF16, "on2")
    wo_st2 = B.sb([128, D], F32, "wost2")
    wo_bf2 = B.sb([128, D], BF16, "wobf2")
    psS = [Ring([PS[0], PS[1]], "psS0"), Ring([PS[2], PS[3]], "psS1")]
    NKT_RUN = int(os.environ.get("NKT", str(NKT)))
    for hd in range(8):
        for r_ in range(4):
            for c_ in range(2):
                S.dma(K01[c_ * 64:(c_ + 1) * 64, c_, r_ * NL:(r_ + 1) * NL],
                      cout_k[hd][r_ * 128 + c_ * 64:r_ * 128 + (c_ + 1) * 64, :], reads=["cout_k%d" % hd, "K01"], writes=["K01"])
            S.dma(V_h[:, r_ * 16:(r_ + 1) * 16, :], cout_v[hd][r_ * NL:(r_ + 1) * NL, :].rearrange("(j p) v -> p j v", p=128),
                  reads=["cout_v%d" % hd, "Vh"], writes=["Vh"])
        for c_ in range(2):
            B.cp("pool", K01[c_ * 64:(c_ + 1) * 64, c_, 4 * NL:4 * NL + NCX], kctx[c_ * 64:(c_ + 1) * 64, hd, :], ["kctx", "K01"], ["K01"])
        B.cp("pool", V_h[:, 64:66, :], vctx[:, hd, :, :], ["vctx", "Vh"], ["Vh"])
        S.dma(wo_st2[:], dw_out_d[hd * 128:(hd + 1) * 128, :], writes=["wost2"])
        B.cp("pool", wo_bf2[:], wo_st2[:], ["wost2"], ["wobf2"])
        for qt in range(NL // 512):
            qc = slice(qt * 512, (qt + 1) * 512)
            for kt in range(NKT_RUN):
                kc = slice(kt * 128, (kt + 1) * 128)
                first, last = kt == 0, kt == NKT_RUN - 1
                for c_ in range(2):
                    ps_, psk = psS[c_].next()
                    B.mm(ps_[:], K01[:, c_, kc], q_all[:, hd, qc], True, True, ["K01", "qall%d" % hd], [psk])
                    et, etk = er.next()
                    B.act(et[:], ps_[:], AF.Exp, [psk], [etk], scale=DIFF_SCALE)
                    B.mm(PS[4 + c_][:], V_h[:, kt, :], et[:], first, last, ["Vh", etk], ["ps%d" % (4 + c_)])
                    B.mm(PS[6 + c_][:], ones_b[:], et[:], first, last, ["onesb", etk], ["ps%d" % (6 + c_)])
            oc = []
            for c_ in range(2):
                rc, rck = ftmp.next()
                S.op("dve", lambda e, o=rc[:], i_=PS[6 + c_][:]: e.reciprocal(out=o, in_=i_), reads=["ps%d" % (6 + c_)], writes=[rck])
                B.tt("dve", rc[:], PS[4 + c_][:], rc[:], ALU.mult, ["ps%d" % (4 + c_), rck], [rck])
                oc.append((rc, rck))
            o_, ok_ = ftmp.next()
            B.stt("dve", o_[:], oc[1][0][:], nlam[:, 0:1], oc[0][0][:], ALU.mult, ALU.add, [oc[1][1], oc[0][1], "nlam"], [ok_])
            sq, sqk = er.next()
            B.act(sq[:], o_[:], AF.Square, [ok_], [sqk])
            ps_, psk = psS[0].next()
            B.mm(ps_[:], m128_b[:], sq[:], True, True, [sqk, "m128b"], [psk])
            rs, rsk = ftmp.next()
            B.rsqrt(rs[:], ps_[:], [psk], rsk)
            B.tt("dve", o_[:], o_[:], rs[:], ALU.mult, [ok_, rsk], [ok_])
            on, onk = onr2.next()
            B.ts("dve", on[:], o_[:], nlam[:, 1:2], None, ALU.mult, None, [ok_, "nlam"], [onk])
            for dk in range(KD):
                ps_, psk = psS[dk % 2].next()
                B.mm(ps_[:], wo_bf2[:, dk * 128:(dk + 1) * 128], on[:], True, True, ["wobf2", onk], [psk])
                B.stt("dve", x_sb[:, dk, qc], ps_[:], modT[:, 1, 16 + dk, 0:1], x_sb[:, dk, qc], ALU.mult, ALU.add,
                      [psk, "modT", "x%d" % dk], ["x%d" % dk])
    B.release(ma)
    if stage == 4:
        return finish("x")

    moe_layer(1, False)

    mf = B.mark()
    sqr = B.ring(3, [128, 512], BF16, "fsq")
    rsr = B.ring(2, [128, 512], F32, "frs")
    otr = B.ring(3, [128, 512], F32, "fout")
    for (c0, cw, isctx) in col_tiles(False):
        for k in range(KD):
            sq, sqk = sqr.next()
            B.act(sq[:], x_sb[:, k, c0:c0 + cw], AF.Square, ["x%d" % k], [sqk])
            B.mm(PS[1][:], mean_b[:], sq[:], k == 0, k == KD - 1, [sqk, "meanb"], ["ps1"])
        rs, rsk = rsr.next()
        B.rsqrt(rs[:], PS[1][:], ["ps1"], rsk)
        for k in range(KD):
            ot, otk = otr.next()
            B.stt("dve", ot[:], x_sb[:, k, c0:c0 + cw], vecs[:, 32 + k:33 + k], rs[:], ALU.mult, ALU.mult,
                  ["x%d" % k, "vecs", rsk], [otk])
            S.dma(out_d[k * 128:(k + 1) * 128, c0:c0 + cw], ot[:], reads=[otk], writes=["out%d" % k])
    S.wait_all("sp", ["out%d" % k for k in range(KD)])
    if dbg:
        for k in range(KD):
            S.dma(dbg_d[k * 128:(k + 1) * 128, :], x_sb[:, k, :], reads=["x%d" % k], writes=["dbg%d" % k])
        S.wait_all("sp", ["dbg%d" % k for k in range(KD)])
    S.emit()
    return nc

    return finish("x")


def _consts():
    ident = np.eye(128, dtype=np.float32)
    ones = np.ones((128, 128), np.float32)
    PT = np.zeros((128, 128), np.float32)
    for p in range(128):
        j = p % 64
        if j < 32:
            PT[p + 32, p] = -1.0
        else:
            PT[p - 32, p] = 1.0
    cmat = np.stack([ident, ones, PT, np.zeros((128, 128), np.float32)], axis=1)
    s = np.arange(128)[:, None]
    t = np.arange(128)[None, :]
    same = (s // 64) == (t // 64)
    mfw = (same & (s <= t)).astype(np.int32)
    mbw = (same & (s >= t)).astype(np.int32)
    masks = np.stack([mfw, mbw], axis=1)
    scanm = np.ones((128, 512), np.float32)
    scanm[:, ::64] = 0.0
    cm2 = np.zeros((128, 18, 128), np.float32)
    for e in range(16):
        cm2[e, e, :] = 1.0
    cm2[0:16, 16, :] = 1.0
    for p in range(80):
        for q in range(80):
            if (p < 64 and q < 64 and p % 16 == q % 16) or (p >= 64 and q == p):
                cm2[p, 17, q] = 1.0
    _consts.cm2 = cm2
    return np.ascontiguousarray(cmat), np.ascontiguousarray(masks), scanm


def make_in_maps(inputs):
    f = lambda a: np.ascontiguousarray(np.asarray(a, dtype=np.float32))
    x, c, ctx, c_ctx = f(inputs["x"]), f(inputs["c"]), f(inputs["ctx"]), f(inputs["c_ctx"])
    cmat, masks, scanm = _consts()
    vecs = np.zeros((128, 64), np.float32)
    pk = lambda v: np.asarray(v, np.float32).reshape(8, 128).T
    nm, nf = f(inputs["norm_mix"]), f(inputs["norm_ffn"])
    vecs[:, 0:8] = pk(nm[0]); vecs[:, 8:16] = pk(nm[1])
    vecs[:, 16:24] = pk(nf[0]); vecs[:, 24:32] = pk(nf[1])
    vecs[:, 32:40] = pk(f(inputs["norm_final"]))
    lbl = f(inputs["hgrn_lb_logits"])
    vecs[:, 40:48] = pk(lbl[0]); vecs[:, 48:56] = pk(lbl[1])
    vecs[:, 56] = f(inputs["hgrn_norm"])[0]
    vecs[:, 57] = f(inputs["diff_subln"])[0]
    vecs[0:64, 58] = float(CAP_L)
    vecs[64:128, 58] = float(CAP_C)
    adab = np.ascontiguousarray(f(inputs["ada_b"]).reshape(2, 48, 128).transpose(2, 0, 1))
    shared = {
        "ada_w": f(inputs["ada_w"]), "adab": adab, "vecs": vecs, "cmat": cmat, "masks": masks, "scanm": scanm,
        "hgrn_w_in": f(inputs["hgrn_w_in"])[0], "hgrn_w_out": f(inputs["hgrn_w_out"])[0],
        "diff_w_in": f(inputs["diff_w_in"])[0], "diff_w_out": f(inputs["diff_w_out"])[0],
        "lvb": np.ascontiguousarray(np.tile(f(inputs["diff_lambda"])[0].reshape(1, 256), (128, 1))),
        "cm2": _consts.cm2, "moe_router": f(inputs["moe_router"]), "moe_w_gate": f(inputs["moe_w_gate"]),
        "moe_w_up": f(inputs["moe_w_up"]), "moe_w_down": f(inputs["moe_w_down"]),
    }
    in_maps = []
    for core in range(NCORES):
        b, seg = core // 4, core % 4
        xT = np.ascontiguousarray(np.concatenate([x[b, seg * NL:(seg + 1) * NL, :], ctx[b]], axis=0).T)
        cT = np.ascontiguousarray(np.stack([c[b], c_ctx], axis=1))
        segm = np.zeros((128, 16), np.float32)
        for s in range(4):
            segm[:, s] = 1.0 if s < seg else 0.0
            segm[:, 4 + s] = 1.0 if s > seg else 0.0
        segm[:, 8:16] = 1.0 - segm[:, 0:8]
        tg = seg * NL + np.arange(NL)
        freq = (10000.0 ** (-np.arange(16, dtype=np.float32) / 16.0)).astype(np.float32)
        ang = np.concatenate([(tg // 64).astype(np.float32)[:, None] * freq, (tg % 64).astype(np.float32)[:, None] * freq], axis=-1)
        pj = np.arange(128) % 32
        rope = np.ascontiguousarray(np.stack([np.cos(ang)[:, pj].T, np.sin(ang)[:, pj].T], axis=1).astype(np.float32))
        m = dict(shared)
        m.update({"xT": xT, "cT": cT, "segm": segm, "rope": rope})
        in_maps.append(m)
    return in_maps


def run(inputs, stage=99, dbg=False):
    nc = build(stage, dbg)
    in_maps = make_in_maps(inputs)
    import os
    n1 = int(os.environ.get("NC1", "0"))
    if n1:
        return run_bass_kernel_spmd(nc, in_maps[:n1], core_ids=list(range(n1)))
    res = run_bass_kernel_spmd(nc, in_maps, core_ids=list(range(NCORES)))
    return res


def kernel(**inputs):
    res = run(inputs, stage=99)
    out = np.zeros((2, 8192, D), np.float32)
    for core in range(NCORES):
        b, seg = core // 4, core % 4
        out[b, seg * NL:(seg + 1) * NL, :] = res.results[core]["outT"].T
    return out
```

```python
from contextlib import ExitStack
import math
import numpy as np
import ml_dtypes
import concourse.bass as bass
import concourse.mybir as mybir
from concourse.bass_utils import run_bass_kernel_spmd

F32 = mybir.dt.float32
BF16 = mybir.dt.bfloat16
I32 = mybir.dt.int32
AF = mybir.ActivationFunctionType
ALU = mybir.AluOpType
AX = mybir.AxisListType

NCORES = 8
D = 1024
KD = 8
NL = 2048
NCX = 256
NT = NL + NCX
EPS = 1e-6
NE = 16
DEXP = 2816
NF = DEXP // 128
CAP_L = 1024
CAP_C = 32
DIFF_SCALE = 0.125

COMPUTE = ("pe", "act", "dve", "pool")
QUEUES = ("sp",)


class Sched:
    def __init__(self, nc):
        self.nc = nc
        self.streams = {e: [] for e in COMPUTE + QUEUES}
        self.cnt = {}
        self.seen = {e: {} for e in COMPUTE + QUEUES}
        self.last_w = {}
        self.readers = {}
        self.n_ins = 0
        self.n_wait = 0

    def _deps(self, reads, writes):
        deps = {}

        def add(sk, n):
            if deps.get(sk, 0) < n:
                deps[sk] = n

        for r in reads:
            lw = self.last_w.get(r)
            if lw:
                add(*lw)
        for w in writes:
            lw = self.last_w.get(w)
            if lw:
                add(*lw)
            for sk, n in self.readers.get(w, {}).items():
                add(sk, n)
        return deps

    def _emit_waits(self, eng, deps, skip_self=False):
        for sk, n in deps.items():
            if skip_self and sk == eng:
                continue
            if self.seen[eng].get(sk, 0) >= n:
                continue
            self.seen[eng][sk] = n
            self.streams[eng].append(("wait", sk, n))
            self.n_wait += 1

    def _record(self, sk, n, reads, writes):
        for r in reads:
            d = self.readers.setdefault(r, {})
            if d.get(sk, 0) < n:
                d[sk] = n
        for w in writes:
            self.last_w[w] = (sk, n)
            self.readers[w] = {}

    def op(self, eng, fn, reads=(), writes=()):
        deps = self._deps(reads, writes)
        self._emit_waits(eng, deps, skip_self=(eng == "pe"))
        n = self.cnt.get(eng, 0) + 1
        self.cnt[eng] = n
        self.streams[eng].append(("ins", fn, eng, 1))
        self._record(eng, n, reads, writes)
        self.n_ins += 1

    def dma(self, out, in_, reads=(), writes=(), slot=None, q="sp", **kw):
        if slot is None:
            slot = writes[0]
        sk = ("dma", slot)
        deps = self._deps(reads, writes)
        self._emit_waits(q, deps)
        n = self.cnt.get(sk, 0) + 1
        self.cnt[sk] = n
        self.streams[q].append(("ins", lambda e: e.dma_start(out=out, in_=in_, **kw), sk, 16))
        self._record(sk, n, reads, writes)
        self.n_ins += 1

    def coll(self, fn, reads=(), writes=()):
        self.op("pool", fn, reads=reads, writes=writes)

    def wait_all(self, eng, bufkeys):
        self._emit_waits(eng, self._deps(bufkeys, ()))

    def barrier(self):
        snap = dict(self.cnt)
        for eng in COMPUTE + QUEUES:
            self._emit_waits(eng, snap)

    def emit(self):
        nc = self.nc
        semkeys = list(self.cnt.keys())
        assert len(semkeys) <= 100, len(semkeys)
        with ExitStack() as st:
            sems = {}
            for i, sk in enumerate(semkeys):
                sems[sk] = st.enter_context(nc.semaphore("s%d" % i))
            block = st.enter_context(nc.Block())

            def run(eng_name):
                def body(e):
                    for item in self.streams[eng_name]:
                        if item[0] == "wait":
                            _, sk, n = item
                            mult = 16 if isinstance(sk, tuple) else 1
                            e.wait_ge(sems[sk], n * mult)
                        else:
                            _, fn, sk, inc = item
                            fn(e).then_inc(sems[sk], inc)
                return body

            block.tensor(run("pe"))
            block.scalar(run("act"))
            block.vector(run("dve"))
            block.gpsimd(run("pool"))
            block.sync(run("sp"))


class Ring:
    def __init__(self, aps, name):
        self.aps = aps
        self.name = name
        self.i = 0

    def next(self):
        j = self.i % len(self.aps)
        self.i += 1
        return self.aps[j], "%s%d" % (self.name, j)


class Builder:
    SB_BASE = 16512
    SB_TOP = 229344

    def __init__(self, stage, dbg):
        self.stage = stage
        self.dbg = dbg
        self.nc = bass.Bass("TRN2", target_bir_lowering=False)
        self.S = Sched(self.nc)
        self.off = self.SB_BASE
        self.names = 0
        self.ps_banks = []

    def sb(self, shape, dt, name=None):
        esz = 4 if dt in (F32, I32) else 2
        sz = int(np.prod(shape[1:])) * esz
        sz = (sz + 31) // 32 * 32
        self.names += 1
        nm = "%s_%d" % (name or "t", self.names)
        t = self.nc.alloc_sbuf_tensor_at(nm, list(shape), dt, offset=self.off)
        self.off += sz
        assert self.off <= self.SB_TOP, ("SBUF overflow", nm, self.off)
        return t

    def mark(self):
        return self.off

    def release(self, mark):
        self.S.barrier()
        self.off = mark

    def ring(self, n, shape, dt, name):
        return Ring([self.sb(shape, dt, name) for _ in range(n)], name + "_%d_" % self.names)

    def dram_in(self, name, shape, dt=F32):
        return self.nc.dram_tensor(name, list(shape), dt, kind="ExternalInput").ap()

    def dram_out(self, name, shape, dt=F32):
        return self.nc.dram_tensor(name, list(shape), dt, kind="ExternalOutput").ap()

    def dram_tmp(self, name, shape, dt=F32):
        return self.nc.dram_tensor(name, list(shape), dt).ap()

    def mm(self, out, lhsT, rhs, start, stop, r, w):
        self.S.op("pe", lambda e: e.matmul(out, lhsT=lhsT, rhs=rhs, start=start, stop=stop), reads=r, writes=w)

    def tr(self, out, in_, ident, r, w):
        self.S.op("pe", lambda e: e.transpose(out, in_, ident), reads=r, writes=w)

    def act(self, out, in_, func, r, w, scale=1.0, bias=0.0):
        self.S.op("act", lambda e: e.activation(out=out, in_=in_, func=func, bias=bias, scale=scale), reads=r, writes=w)

    def tt(self, eng, out, a, b_, op, r, w):
        self.S.op(eng, lambda e: e.tensor_tensor(out=out, in0=a, in1=b_, op=op), reads=r, writes=w)

    def ts(self, eng, out, a, s1, s2, op0, op1, r, w):
        if s2 is None:
            self.S.op(eng, lambda e: e.tensor_scalar(out=out, in0=a, scalar1=s1, scalar2=None, op0=op0), reads=r, writes=w)
        else:
            self.S.op(eng, lambda e: e.tensor_scalar(out=out, in0=a, scalar1=s1, scalar2=s2, op0=op0, op1=op1), reads=r, writes=w)

    def stt(self, eng, out, in0, scalar, in1, op0, op1, r, w):
        self.S.op(eng, lambda e: e.scalar_tensor_tensor(out=out, in0=in0, scalar=scalar, in1=in1, op0=op0, op1=op1), reads=r, writes=w)

    def cp(self, eng, out, in_, r, w):
        if eng == "act":
            self.S.op("act", lambda e: e.copy(out=out, in_=in_), reads=r, writes=w)
        else:
            self.S.op(eng, lambda e: e.tensor_copy(out=out, in_=in_), reads=r, writes=w)

    def rsqrt(self, out, in_, r, wk, eps=EPS):
        self.act(out, in_, AF.Sqrt, list(r) + ["epsT"], [wk], bias=self.eps_ap(eps))
        self.S.op("dve", lambda e: e.reciprocal(out=out, in_=out), reads=[wk], writes=[wk])

    def eps_ap(self, eps):
        return self.epsT[:, 0:1]

    def memset(self, eng, ap, val, w):
        self.S.op(eng, lambda e: e.memset(ap, val), reads=(), writes=w)


def col_tiles(with_ctx=True):
    t = [(i * 512, 512, False) for i in range(NL // 512)]
    if with_ctx:
        t.append((NL, NCX, True))
    return t


def build(stage=99, dbg=False):
    B = Builder(stage, dbg)
    nc, S = B.nc, B.S

    xT_d = B.dram_in("xT", [D, NT])
    cT_d = B.dram_in("cT", [D, 2])
    ada_w_d = B.dram_in("ada_w", [2, D, 6 * D])
    adab_d = B.dram_in("adab", [128, 2, 48])
    vecs_d = B.dram_in("vecs", [128, 64])
    cmat_d = B.dram_in("cmat", [128, 4, 128])
    masks_d = B.dram_in("masks", [128, 2, 128], I32)
    scanm_d = B.dram_in("scanm", [128, 512])
    segm_d = B.dram_in("segm", [128, 16])
    hw_in_d = B.dram_in("hgrn_w_in", [D, 5 * D])
    hw_out_d = B.dram_in("hgrn_w_out", [D, D])
    dw_in_d = B.dram_in("diff_w_in", [D, 3 * D])
    dw_out_d = B.dram_in("diff_w_out", [D, D])
    lvb_d = B.dram_in("lvb", [128, 256])
    rope_d = B.dram_in("rope", [128, 2, NL])
    cm2_d = B.dram_in("cm2", [128, 18, 128])
    router_d = B.dram_in("moe_router", [2, D, NE])
    wgate_d = B.dram_in("moe_w_gate", [2, NE, D, DEXP])
    wup_d = B.dram_in("moe_w_up", [2, NE, D, DEXP])
    wdown_d = B.dram_in("moe_w_down", [2, NE, DEXP, D])
    out_d = B.dram_out("outT", [D, NL])
    dbg_d = B.dram_out("dbg", [D, NT]) if dbg else None

    x_sb = B.sb([128, KD, NT], F32, "x")
    h_sb = B.sb([128, KD, NT], BF16, "h")
    cmat = B.sb([128, 4, 128], F32, "cmat")
    ident_f = cmat[:, 0, :]
    ident_b = B.sb([128, 128], BF16, "identb")
    ones_b = B.sb([128, 128], BF16, "onesb")
    mean_b = B.sb([128, 128], BF16, "meanb")
    m128_b = B.sb([128, 128], BF16, "m128b")
    masks = B.sb([128, 2, 128], I32, "masks")
    scanm = B.sb([128, 512], F32, "scanm")
    segm = B.sb([128, 16], F32, "segm")
    vecs = B.sb([128, 64], F32, "vecs")
    modT = B.sb([128, 2, 48, 2], F32, "modT")
    adab = B.sb([128, 2, 48], F32, "adab")
    coefA = B.sb([128, 4, 2, KD], F32, "coefA")
    siluc = B.sb([128, KD, 2], F32, "siluc")
    lb = B.sb([128, 8], F32, "lb")
    B.epsT = B.sb([128, 2], F32, "epsT")
    B.memset("pool", B.epsT[:], EPS, ["epsT"])
    oml = B.sb([128, 8], F32, "oml")

    PS = [nc.alloc_psum_tensor("ps%d" % i, [128, 512], F32) for i in range(8)]

    for k in range(KD):
        S.dma(x_sb[:, k, :], xT_d[k * 128:(k + 1) * 128, :], writes=["x%d" % k])
    S.dma(cmat[:], cmat_d, writes=["cmat"])
    S.dma(masks[:], masks_d, writes=["masks"])
    S.dma(scanm[:], scanm_d, writes=["scanm"])
    S.dma(segm[:], segm_d, writes=["segm"])
    S.dma(vecs[:], vecs_d, writes=["vecs"])
    S.dma(adab[:], adab_d, writes=["adab"])
    S.dma(siluc[:], cT_d.rearrange("(k p) c -> p k c", p=128), writes=["siluc"])
    B.cp("dve", ident_b[:], cmat[:, 0, :], ["cmat"], ["identb"])
    B.cp("dve", ones_b[:], cmat[:, 1, :], ["cmat"], ["onesb"])
    B.ts("dve", mean_b[:], cmat[:, 1, :], 1.0 / D, None, ALU.mult, None, ["cmat"], ["meanb"])
    B.ts("dve", m128_b[:], cmat[:, 1, :], 1.0 / 128, None, ALU.mult, None, ["cmat"], ["m128b"])
    B.act(siluc[:], siluc[:], AF.Silu, ["siluc"], ["siluc"])
    B.tt("dve", lb[:], vecs[:, 40:48], vecs[:, 48:56], ALU.subtract, ["vecs"], ["lb"])
    B.act(lb[:], lb[:], AF.Sigmoid, ["lb"], ["lb"])
    B.ts("dve", oml[:], lb[:], -1.0, 1.0, ALU.mult, ALU.add, ["lb"], ["oml"])

    m0 = B.mark()
    adaring = B.ring(2, [128, KD, 768], F32, "adaw")
    for i in range(2):
        for blk in range(8):
            wt, wk = adaring.next()
            S.dma(wt[:], ada_w_d[i, :, blk * 768:(blk + 1) * 768].rearrange("(k p) j -> p k j", p=128), writes=[wk])
            for jj in range(6):
                j = blk * 6 + jj
                for k in range(KD):
                    B.mm(PS[0][:, j * 2:(j + 1) * 2], wt[:, k, jj * 128:(jj + 1) * 128], siluc[:, k, :],
                         k == 0, k == KD - 1, [wk, "siluc"], ["ps0"])
        for col in range(2):
            B.tt("dve", modT[:, i, :, col], PS[0][:, 0:96].rearrange("p (j c) -> p j c", c=2)[:, :, col],
                 adab[:, i, :], ALU.add, ["ps0", "adab"], ["modT"])
    for n in range(4):
        layer, ffn = n // 2, n % 2
        gcol = (16 if ffn else 0) + 8 * layer
        sc0 = 32 if ffn else 8
        for col in range(2):
            B.stt("dve", coefA[:, n, col, :], modT[:, layer, sc0:sc0 + 8, col], 1.0, vecs[:, gcol:gcol + 8],
                  ALU.add, ALU.mult, ["modT", "vecs"], ["coefA"])
    B.release(m0)

    def norm_mod(n, with_ctx=True):
        layer, ffn = n // 2, n % 2
        sh0 = 24 if ffn else 0
        mk = B.mark()
        sqr = B.ring(3, [128, 512], BF16, "sq")
        rsr = B.ring(2, [128, 512], F32, "rstd")
        tmr = B.ring(3, [128, 512], F32, "ntmp")
        for (c0, cw, isctx) in col_tiles(with_ctx):
            col = 1 if isctx else 0
            for k in range(KD):
                sq, sqk = sqr.next()
                B.act(sq[:, :cw], x_sb[:, k, c0:c0 + cw], AF.Square, ["x%d" % k], [sqk])
                B.mm(PS[1][:, :cw], mean_b[:], sq[:, :cw], k == 0, k == KD - 1, [sqk, "meanb"], ["ps1"])
            rs, rsk = rsr.next()
            B.rsqrt(rs[:, :cw], PS[1][:, :cw], ["ps1"], rsk)
            for k in range(KD):
                tm, tmk = tmr.next()
                B.tt("dve", tm[:, :cw], x_sb[:, k, c0:c0 + cw], rs[:, :cw], ALU.mult, ["x%d" % k, rsk], [tmk])
                B.act(h_sb[:, k, c0:c0 + cw], tm[:, :cw], AF.Identity, [tmk, "coefA", "modT"], ["h%d" % k],
                      scale=coefA[:, n, col, k:k + 1], bias=modT[:, layer, sh0 + k, col:col + 1])
        B.release(mk)

    norm_mod(0)

    def finish(dump=None):
        if dbg and dump is not None:
            if dump == "x":
                for k in range(KD):
                    S.dma(dbg_d[k * 128:(k + 1) * 128, :], x_sb[:, k, :], reads=["x%d" % k], writes=["dbg%d" % k])
            else:
                mk = B.mark()
                hf = B.sb([128, NT], F32, "hf")
                for k in range(KD):
                    B.cp("dve", hf[:], h_sb[:, k, :], ["h%d" % k], ["hf"])
                    S.dma(dbg_d[k * 128:(k + 1) * 128, :], hf[:], reads=["hf"], writes=["dbg%d" % k])
            S.wait_all("sp", ["dbg%d" % k for k in range(KD)])
        for k in range(KD):
            S.dma(out_d[k * 128:(k + 1) * 128, :], x_sb[:, k, 0:NL], reads=["x%d" % k], writes=["out%d" % k])
        S.wait_all("sp", ["out%d" % k for k in range(KD)])
        S.emit()
        return nc

    if stage == 1:
        return finish("h")

    noml = B.sb([128, 8], F32, "noml")
    B.ts("dve", noml[:], oml[:], -1.0, None, ALU.mult, None, ["oml"], ["noml"])
    cin_h = [B.dram_tmp("cin_h%d" % i, [512, 256]) for i in range(4)]
    cout_h = [B.dram_tmp("cout_h%d" % i, [2048, 256]) for i in range(4)]
    PSB = nc.alloc_psum_tensor("psb", [128, 1024], BF16) if False else None
    PS4b = PS[4][:].bitcast(BF16)

    mh = B.mark()
    wst = B.ring(2, [128, KD, 128], F32, "wst")
    wbf = B.ring(6, [128, KD, 128], BF16, "wbf")
    q_sb = B.sb([128, NT], BF16, "q")
    g_sb = B.sb([128, NT], BF16, "g")
    v_tok = B.sb([128, NT // 128, 128], BF16, "vtok")
    o_acc = B.sb([128, NT], F32, "oacc")
    tmp = B.ring(6, [128, 512], F32, "tmp")
    qbr = B.ring(2, [128, 512], BF16, "qb")
    kbr = B.ring(2, [128, 512], BF16, "kb")
    kdr = B.ring(2, [128, 512], BF16, "kd")
    kdTr = B.ring(2, [128, 2, 4, 128], BF16, "kdT")
    for t_ in kdTr.aps:
        _, k_ = kdTr.next()
        B.memset("pool", t_[:], 0.0, [k_])
    sqr_ = B.ring(2, [128, 8, 128], BF16, "Sq")
    usbr = B.ring(2, [128, 2, 4, 128], F32, "Usb")
    aTf = B.ring(4, [128, 128], BF16, "aTf")
    aTb = B.ring(4, [128, 128], BF16, "aTb")
    for r_ in (aTf, aTb):
        for t_ in r_.aps:
            _, k_ = r_.next()
            B.memset("pool", t_[:], 0.0, [k_])
    srun = B.ring(3, [128, 128], F32, "srun")
    small = B.ring(8, [128, 8], F32, "small")
    lbuf = B.ring(1, [128, 4, 129], F32, "lbuf")
    onr = B.ring(2, [128, 512], BF16, "on")
    wo_st = B.sb([128, D], F32, "wost")
    wo_bf = B.sb([128, D], BF16, "wobf")
    totacc = B.sb([128, 8], F32, "totacc")

    def load_w(j):
        st_, stk = wst.next()
        S.dma(st_[:], hw_in_d[:, j * 128:(j + 1) * 128].rearrange("(k p) c -> p k c", p=128), writes=[stk])
        wb, wbk = wbf.next()
        B.cp("pool", wb[:], st_[:], [stk], [wbk])
        return wb, wbk

    def proj_fm(ps, psk, wb, wbk, c0, cw):
        for k in range(KD):
            B.mm(ps[:, :cw], wb[:, k, :], h_sb[:, k, c0:c0 + cw], k == 0, k == KD - 1, [wbk, "h%d" % k], [psk])

    import os
    HSTOP = int(os.environ.get("HSTOP", "99"))
    HFULL = int(os.environ.get("HFULL", "99"))

    def hgrn_dir(hd, d_, full, wf, wfk):
        lat_tiles = [(i * 512, 512, False) for i in range(4)]
        if d_ == 1:
            lat_tiles = lat_tiles[::-1]
        tiles = ([(NL, NCX, True)] if full else []) + lat_tiles
        S_cur, S_k = srun.next()
        B.memset("pool", S_cur[:], 0.0, [S_k])
        aTr = aTf if d_ == 0 else aTb
        first_lat = True
        ti = 0
        for (c0, cw, isctx) in tiles:
            nch = cw // 64
            nsub = cw // 128
            if full and (not isctx) and first_lat:
                lb_, lbk = lbuf.next()
                for s_ in range(4):
                    p_ = hd * 2 + d_
                    r0 = s_ * 512 + (p_ % 4) * 128
                    S.dma(lb_[:, s_, :], cout_h[p_ // 4][r0:r0 + 128, 0:129], reads=["cout_h%d" % (p_ // 4)], writes=[lbk])
                order = [0, 1, 2, 3] if d_ == 0 else [3, 2, 1, 0]
                for s_ in order:
                    mcol = (0 if d_ == 0 else 4) + s_
                    sm, smk = small.next()
                    B.ts("dve", sm[:, 0:1], lb_[:, s_, 128:129], segm[:, mcol:mcol + 1], segm[:, 8 + mcol:9 + mcol],
                         ALU.mult, ALU.add, [lbk, "segm"], [smk])
                    t1, t1k = tmp.next()
                    B.ts("dve", t1[:, :128], lb_[:, s_, 0:128], segm[:, mcol:mcol + 1], None, ALU.mult, None,
                         [lbk, "segm"], [t1k])
                    S_n, S_nk = srun.next()
                    B.stt("dve", S_n[:], S_cur[:], sm[:, 0:1], t1[:, :128], ALU.mult, ALU.add, [S_k, smk, t1k], [S_nk])
                    S_cur, S_k = S_n, S_nk
            if not isctx:
                first_lat = False
            psF, psFk = (PS[1], "ps1") if d_ == 0 else (PS[2], "ps2")
            proj_fm(psF, psFk, wf, wfk, c0, cw)
            t_sig, k_sig = tmp.next()
            B.act(t_sig[:, :cw], psF[:, :cw], AF.Sigmoid, [psFk], [k_sig])
            t_lf, k_lf = tmp.next()
            B.act(t_lf[:, :cw], t_sig[:, :cw], AF.Ln, [k_sig, "oml", "lb"], [k_lf],
                  scale=oml[:, hd:hd + 1], bias=lb[:, hd:hd + 1])
            t_kk, k_kk = tmp.next()
            B.ts("dve", t_kk[:, :cw], t_sig[:, :cw], noml[:, hd:hd + 1], oml[:, hd:hd + 1], ALU.mult, ALU.add,
                 [k_sig, "noml", "oml"], [k_kk])
            t_b, k_b = tmp.next()
            S.op("dve", lambda e, o=t_b[:, :cw], a=scanm[:, :cw], b_=t_lf[:, :cw]:
                 e.tensor_tensor_scan(out=o, data0=a, data1=b_, initial=0.0, op0=ALU.mult, op1=ALU.add),
                 reads=["scanm", k_lf], writes=[k_b])
            b3 = t_b[:, :cw].rearrange("p (c s) -> p c s", s=64)
            tot, totk = small.next()
            B.cp("dve", tot[:, :nch], b3[:, :, 63], [k_b], [totk])
            tot_bc = tot[:, :nch].unsqueeze(2).broadcast_to([128, nch, 64])
            if d_ == 1:
                t_c, k_c = tmp.next()
                c3 = t_c[:, :cw].rearrange("p (c s) -> p c s", s=64)
                B.tt("dve", c3, tot_bc, b3, ALU.subtract, [totk, k_b], [k_c])
                B.tt("dve", t_c[:, :cw], t_c[:, :cw], t_lf[:, :cw], ALU.add, [k_c, k_lf], [k_c])
            else:
                t_c, k_c, c3 = t_b, k_b, b3
            t_d3, k_d3 = tmp.next()
            d33 = t_d3[:, :cw].rearrange("p (c s) -> p c s", s=64)
            B.tt("dve", d33, tot_bc, c3, ALU.subtract, [totk, k_c], [k_d3])
            B.act(t_d3[:, :cw], t_d3[:, :cw], AF.Exp, [k_d3], [k_d3])
            kd, kdk = kdr.next()
            B.tt("pool", kd[:, :cw], t_kk[:, :cw], t_d3[:, :cw], ALU.mult, [k_kk, k_d3], [kdk])
            Dc, Dck = small.next()
            B.act(Dc[:, :nch], tot[:, :nch], AF.Exp, [totk], [Dck])
            if not full:
                B.S.op("dve", lambda e, o=totacc[:, ti:ti + 1], i_=tot[:, :nch]:
                       e.tensor_reduce(out=o, in_=i_, axis=AX.X, op=ALU.add), reads=[totk], writes=["totacc"])
            if full:
                cmid_bc = c3[:, :, 32:33].broadcast_to([128, nch, 64])
                t_d1, k_d1 = tmp.next()
                d13 = t_d1[:, :cw].rearrange("p (c s) -> p c s", s=64)
                B.tt("dve", d13, c3, cmid_bc, ALU.subtract, [k_c], [k_d1])
                t_e1, k_e1 = tmp.next()
                B.act(t_e1[:, :cw], t_d1[:, :cw], AF.Exp, [k_d1], [k_e1])
                qb, qbk = qbr.next()
                B.tt("pool", qb[:, :cw], q_sb[:, c0:c0 + cw], t_e1[:, :cw], ALU.mult, ["q", k_e1], [qbk])
                t_e2, k_e2 = t_d1, k_d1
                B.act(t_e2[:, :cw], t_d1[:, :cw], AF.Exp, [k_d1], [k_e2], scale=-1.0)
                kb, kbk = kbr.next()
                B.tt("pool", kb[:, :cw], t_kk[:, :cw], t_e2[:, :cw], ALU.mult, [k_kk, k_e2], [kbk])
                ebr, ebrk = small.next()
                B.act(ebr[:, :nch], c3[:, :, 32], AF.Exp, [k_c], [ebrk])
            if HSTOP <= 1:
                continue
            kdT, kdTk = kdTr.next()
            for sj in range(nsub):
                B.tr(PS4b[:, sj * 128:(sj + 1) * 128], kd[:, sj * 128:(sj + 1) * 128], ident_b[:], [kdk, "identb"], ["ps4"])
            for half in range(2):
                pr = slice(half * 64, (half + 1) * 64)
                B.cp("dve", kdT[pr, half, :nsub, :], PS4b[pr, :nsub * 128].rearrange("p (j k) -> p j k", k=128),
                     ["ps4"], [kdTk])
            if HSTOP <= 2:
                continue
            tj0 = c0 // 128
            ubank = []
            for ch in range(nch):
                sj, half = ch // 2, ch % 2
                UB = [int(v) for v in os.environ.get("UB", "5,6").split(",")]
                bank, bk = (PS[UB[0]], "ps%d" % UB[0]) if half == 0 else (PS[UB[1]], "ps%d" % UB[1])
                B.mm(bank[:, sj * 128:(sj + 1) * 128], kdT[:, half, sj, :],
                     v_tok[:, tj0 + sj, :], True, True, [kdTk, "vtok"], [bk + "_%d" % sj])
                ubank.append((bank[:, sj * 128:(sj + 1) * 128], bk + "_%d" % sj))
            if HSTOP <= 3:
                continue
            if True:
                Usb, Usbk = usbr.next()
                UB = [int(v) for v in os.environ.get("UB", "5,6").split(",")]
                for half in range(2):
                    B.cp("dve" if half == 0 else "act", Usb[:, half, :nsub, :],
                         PS[UB[half]][:, :nsub * 128].rearrange("p (j k) -> p j k", k=128),
                         ["ps%d_%d" % (UB[half], j_) for j_ in range(nsub)], [Usbk + "_%d" % half])
                ubank = [(Usb[:, ch % 2, ch // 2, :], Usbk + "_%d" % (ch % 2)) for ch in range(nch)]
            Sq, Sqk = sqr_.next()
            chs = list(range(nch)) if d_ == 0 else list(range(nch))[::-1]
            CHN = int(os.environ.get("CHN", "99"))
            CHMODE = int(os.environ.get("CHMODE", "3"))
            for ch in chs[:CHN]:
                if full:
                    B.ts("pool", Sq[:, ch, :], S_cur[:], ebr[:, ch:ch + 1], None, ALU.mult, None, [S_k, ebrk], [Sqk + "_%d" % ch])
                S_n, S_nk = srun.next()
                if CHMODE & 1:
                    B.ts("dve", S_n[:], S_cur[:], Dc[:, ch:ch + 1], None, ALU.mult, None, [S_k, Dck], [S_nk])
                if CHMODE & 2:
                    B.tt("dve", S_n[:], ubank[ch][0], S_n[:], ALU.add, [ubank[ch][1], S_nk], [S_nk])
                if CHMODE & 4:
                    B.cp("act", S_n[:], ubank[ch][0], [ubank[ch][1]], [S_nk])
                if CHMODE & 8:
                    B.cp("dve", S_n[:], ubank[ch][0], [ubank[ch][1]], [S_nk])
                S_cur, S_k = S_n, S_nk
            if full and HFULL >= 1:
                mask_ap = masks[:, d_, :]
                for sj in range(nsub):
                    cs = slice(sj * 128, (sj + 1) * 128)
                    B.mm(PS[3][:, cs], kb[:, cs], qb[:, cs], True, True, [kbk, qbk], ["ps3"])
                aTs = []
                for sj in range(nsub):
                    cs = slice(sj * 128, (sj + 1) * 128)
                    aT, aTk = aTr.next()
                    S.op("dve", lambda e, o=aT[:], m_=mask_ap, d__=PS[3][:, cs]: e.copy_predicated(out=o, mask=m_, data=d__),
                         reads=["ps3", "masks"], writes=[aTk])
                    aTs.append((aT, aTk))
                for sj in range(nsub):
                    cs = slice(sj * 128, (sj + 1) * 128)
                    aT, aTk = aTs[sj]
                    B.mm(PS[7][:, cs], v_tok[:, tj0 + sj, :], aT[:], True, False, ["vtok", aTk], ["ps7"])
                    for half in range(2):
                        ch = sj * 2 + half
                        cc = slice(ch * 64, (ch + 1) * 64)
                        B.mm(PS[7][:, cc], Sq[:, ch, :], qb[:, cc], False, half == 1, [Sqk + "_%d" % ch, qbk], ["ps7"])
                if d_ == 0:
                    B.cp("act", o_acc[:, c0:c0 + cw], PS[7][:, :cw], ["ps7"], ["oacc%d" % (c0 // 512)])
                else:
                    B.tt("dve", o_acc[:, c0:c0 + cw], o_acc[:, c0:c0 + cw], PS[7][:, :cw], ALU.add,
                         ["ps7", "oacc%d" % (c0 // 512)], ["oacc%d" % (c0 // 512)])
            ti += 1
        return S_cur, S_k

    for hd in range({14: 0, 16: 1}.get(stage, 8)):
        wv, wvk = load_w(24 + hd)
        wfs = [load_w(8 + hd), load_w(16 + hd)]
        for tj in range(NL // 128):
            bank_col = (tj % 4) * 128
            for k in range(KD):
                B.mm(PS[3][:, bank_col:bank_col + 128], h_sb[:, k, tj * 128:(tj + 1) * 128], wv[:, k, :],
                     k == 0, k == KD - 1, ["h%d" % k, wvk], ["ps3"])
            if tj % 4 == 3:
                B.cp("act", v_tok[:, tj - 3:tj + 1, :], PS[3][:].rearrange("p (j v) -> p j v", v=128), ["ps3"], ["vtok"])
        for d_ in range(2):
            S_f, S_fk = hgrn_dir(hd, d_, False, wfs[d_][0], wfs[d_][1])
            if HSTOP <= 4:
                continue
            p_ = hd * 2 + d_
            ci, r0 = p_ // 4, (p_ % 4) * 128
            S.dma(cin_h[ci][r0:r0 + 128, 0:128], S_f[:], reads=[S_fk], writes=["cin_h%d" % ci], slot="cin_hs%d" % (d_))
            sm, smk = small.next()
            S.op("dve", lambda e, o=sm[:, 0:1], i_=totacc[:, 0:4]: e.tensor_reduce(out=o, in_=i_, axis=AX.X, op=ALU.add),
                 reads=["totacc"], writes=[smk])
            B.act(sm[:, 1:2], sm[:, 0:1], AF.Exp, [smk], [smk])
            S.dma(cin_h[ci][r0:r0 + 128, 128:129], sm[:, 1:2], reads=[smk], writes=["cin_h%d" % ci], slot="cin_hd%d" % (d_),
                  allow_slow_non_contiguous=True)
    import os
    if not os.environ.get("NOCC"):
        for ci in range(4):
            S.coll(lambda e, ci=ci: e.collective_compute("AllGather", ALU.bypass, replica_groups=[[0, 1, 2, 3], [4, 5, 6, 7]],
                                                        ins=[cin_h[ci]], outs=[cout_h[ci]]),
                 reads=["cin_h%d" % ci], writes=["cout_h%d" % ci])

    if not os.environ.get("NOCC"):
        cin_zh = B.dram_tmp("cin_zh", [16, 64])
        cout_zh = B.dram_tmp("cout_zh", [64, 64])
        S.dma(cin_zh, segm[0:16, 0:16].bitcast(F32) if False else vecs[0:16, 0:64], reads=["vecs"] + ["cout_h%d" % i for i in range(4)], writes=["cin_zh"])
        S.coll(lambda e: e.collective_compute("AllGather", ALU.bypass, replica_groups=[[0, 1, 2, 3], [4, 5, 6, 7]],
                                              ins=[cin_zh], outs=[cout_zh]),
               reads=["cin_zh"] + ["cout_h%d" % i for i in range(4)], writes=["cout_zh"] + ["cout_h%d" % i for i in range(4)])
    if stage in (14, 15, 16):
        return finish("x")
    for hd in range(8):
        wq, wqk = load_w(hd)
        wg, wgk = load_w(32 + hd)
        wv, wvk = load_w(24 + hd)
        wfs = [load_w(8 + hd), load_w(16 + hd)]
        S.dma(wo_st[:], hw_out_d[hd * 128:(hd + 1) * 128, :], writes=["wost"])
        B.cp("pool", wo_bf[:], wo_st[:], ["wost"], ["wobf"])
        for (c0, cw, isctx) in col_tiles():
            proj_fm(PS[0], "ps0", wq, wqk, c0, cw)
            B.act(q_sb[:, c0:c0 + cw], PS[0][:, :cw], AF.Silu, ["ps0"], ["q"])
            proj_fm(PS[0], "ps0", wg, wgk, c0, cw)
            B.act(g_sb[:, c0:c0 + cw], PS[0][:, :cw], AF.Silu, ["ps0"], ["g"])
        for tj in range(NT // 128):
            bank_col = (tj % 4) * 128
            for k in range(KD):
                B.mm(PS[3][:, bank_col:bank_col + 128], h_sb[:, k, tj * 128:(tj + 1) * 128], wv[:, k, :],
                     k == 0, k == KD - 1, ["h%d" % k, wvk], ["ps3"])
            if tj % 4 == 3 or tj == NT // 128 - 1:
                n_ = tj % 4 + 1
                B.cp("act", v_tok[:, tj - n_ + 1:tj + 1, :], PS[3][:, :n_ * 128].rearrange("p (j v) -> p j v", v=128),
                     ["ps3"], ["vtok"])
        for d_ in range(2):
            hgrn_dir(hd, d_, True, wfs[d_][0], wfs[d_][1])
        for (c0, cw, isctx) in (col_tiles() if HFULL >= 2 else []):
            col = 1 if isctx else 0
            ok = "oacc%d" % (c0 // 512)
            sq, sqk = qbr.next()
            B.act(sq[:, :cw], o_acc[:, c0:c0 + cw], AF.Square, [ok], [sqk])
            B.mm(PS[1][:, :cw], m128_b[:], sq[:, :cw], True, True, [sqk, "m128b"], ["ps1"])
            rs, rsk = tmp.next()
            B.rsqrt(rs[:, :cw], PS[1][:, :cw], ["ps1"], rsk)
            t1, t1k = tmp.next()
            B.tt("dve", t1[:, :cw], o_acc[:, c0:c0 + cw], rs[:, :cw], ALU.mult, [ok, rsk], [t1k])
            on, onk = onr.next()
            B.stt("dve", on[:, :cw], t1[:, :cw], vecs[:, 56:57], g_sb[:, c0:c0 + cw], ALU.mult, ALU.mult,
                  [t1k, "vecs", "g"], [onk])
            for dk in range(KD):
                bank, bk = (PS[0], "ps0") if dk % 2 == 0 else (PS[2], "ps2")
                B.mm(bank[:, :cw], wo_bf[:, dk * 128:(dk + 1) * 128], on[:, :cw], True, True, ["wobf", onk], [bk])
                B.stt("dve", x_sb[:, dk, c0:c0 + cw], bank[:, :cw], modT[:, 0, 16 + dk, col:col + 1],
                      x_sb[:, dk, c0:c0 + cw], ALU.mult, ALU.add, [bk, "modT", "x%d" % dk], ["x%d" % dk])
    B.release(mh)

    if stage == 2:
        return finish("x")

    def moe_layer(L, with_ctx):
        n = 2 * L + 1
        norm_mod(n, with_ctx)
        mk = B.mark()
        cm2 = B.sb([128, 18, 128], F32, "cm2")
        S.dma(cm2[:], cm2_d, writes=["cm2"])
        tiles = col_tiles(with_ctx)
        ncols = NT if with_ctx else NL
        gw = B.sb([128, NT], F32, "gw")
        mk2 = B.mark()
        wr_st = B.sb([128, KD, NE], F32, "wrst")
        wr_bf = B.sb([128, KD, 128], BF16, "wrbf")
        S.dma(wr_st[:], router_d[L].rearrange("(k p) e -> p k e", p=128), writes=["wrst"])
        B.memset("pool", wr_bf[:], 0.0, ["wrbf"])
        B.cp("pool", wr_bf[:, :, 0:NE], wr_st[:], ["wrst", "wrbf"], ["wrbf"])
        etile = B.ring(2, [128, 512], F32, "etile")
        for (c0, cw, isctx) in tiles:
            for k in range(KD):
                B.mm(PS[0][:, :cw], wr_bf[:, k, :], h_sb[:, k, c0:c0 + cw], k == 0, k == KD - 1, ["wrbf", "h%d" % k], ["ps0"])
            et, etk = etile.next()
            B.act(et[:, :cw], PS[0][:, :cw], AF.Exp, ["ps0"], [etk])
            B.mm(PS[1][:, :cw], cm2[:, 16, :], et[:, :cw], True, True, ["cm2", etk], ["ps1"])
            rt, rtk = etile.next()
            S.op("dve", lambda e, o=rt[:, :cw], i_=PS[1][:, :cw]: e.reciprocal(out=o, in_=i_), reads=["ps1"], writes=[rtk])
            B.tt("dve", gw[:, c0:c0 + cw], et[:, :cw], rt[:, :cw], ALU.mult, [etk, rtk], ["gw"])
        cin_a = B.dram_tmp("cin_a%d" % L, [NE, NL])
        cout_a = B.dram_tmp("cout_a%d" % L, [4 * NE, NL])
        S.dma(cin_a, gw[0:NE, 0:NL], reads=["gw"], writes=["cin_a"])
        S.coll(lambda e: e.collective_compute("AllGather", ALU.bypass, replica_groups=[[0, 1, 2, 3], [4, 5, 6, 7]],
                                              ins=[cin_a], outs=[cout_a]), reads=["cin_a"], writes=["cout_a"])
        cin_z = B.dram_tmp("cin_z%d" % L, [NE, 64])
        cout_z = B.dram_tmp("cout_z%d" % L, [4 * NE, 64])
        S.dma(cin_z, gw[0:NE, 0:64], reads=["gw", "cout_a"], writes=["cin_z"])
        S.coll(lambda e: e.collective_compute("AllGather", ALU.bypass, replica_groups=[[0, 1, 2, 3], [4, 5, 6, 7]],
                                              ins=[cin_z], outs=[cout_z]), reads=["cin_z", "cout_a"], writes=["cout_z", "cout_a"])
        A = B.sb([128, NL], F32, "A")
        B.memset("pool", A[:], 0.0, ["A"])
        S.dma(A[0:64, :], cout_a, reads=["cout_a", "A"], writes=["A"])
        if with_ctx:
            S.dma(A[64:80, 0:NCX], gw[0:NE, NL:NT], reads=["gw", "A"], writes=["A"])
        msk = B.sb([128, NL], F32, "msk")
        thr = B.sb([128, 1], F32, "thr")
        B.memset("pool", thr[:], 0.0, ["thr"])
        sm_ = B.ring(4, [128, 2], F32, "thsm")
        sm8 = B.ring(2, [128, 8], F32, "thsm8")
        for it in range(1, 31):
            step = 2.0 ** (-it)
            cand, ck = sm_.next()
            B.ts("dve", cand[:, 0:1], thr[:], step, None, ALU.add, None, ["thr"], [ck])
            B.ts("dve", msk[:], A[:], cand[:, 0:1], None, ALU.is_ge, None, ["A", ck], ["msk"])
            c8, c8k = sm8.next()
            S.op("dve", lambda e, o=c8[:]: e.tensor_reduce(out=o, in_=msk[:].rearrange("p (g c) -> p g c", c=256),
                                                          axis=AX.X, op=ALU.add), reads=["msk"], writes=[c8k])
            B.mm(PS[2][:, 0:8], cm2[:, 17, :], c8[:], True, True, ["cm2", c8k], ["ps2"])
            S.op("dve", lambda e, o=cand[:, 1:2]: e.tensor_reduce(out=o, in_=PS[2][:, 0:8], axis=AX.X, op=ALU.add),
                 reads=["ps2"], writes=[ck])
            ge, gk = sm_.next()
            B.tt("dve", ge[:, 0:1], cand[:, 1:2], vecs[:, 58:59], ALU.is_ge, [ck, "vecs"], [gk])
            B.stt("dve", thr[:], ge[:, 0:1], step, thr[:], ALU.mult, ALU.add, [gk, "thr"], ["thr"])
        B.stt("dve", gw[0:NE, 0:NL], gw[0:NE, 0:NL], thr[0:NE, 0:1], gw[0:NE, 0:NL], ALU.is_ge, ALU.mult, ["gw", "thr"], ["gw"])
        if with_ctx:
            thc = B.sb([128, 1], F32, "thc")
            S.dma(thc[0:NE, 0:1], thr[64:80, 0:1], reads=["thr"], writes=["thc"])
            B.stt("dve", gw[0:NE, NL:NT], gw[0:NE, NL:NT], thc[0:NE, 0:1], gw[0:NE, NL:NT], ALU.is_ge, ALU.mult,
                  ["gw", "thc"], ["gw"])
        B.ts("pool", gw[:], gw[:], cm2[:, 16, 0:1], None, ALU.mult, None, ["gw", "cm2"], ["gw"])
        B.release(mk2)
        gwbr = B.ring(2, [128, NT], F32, "gwb")
        wgs = B.ring(2, [128, KD, 128], F32, "wgs")
        wus = B.ring(2, [128, KD, 128], F32, "wus")
        wds = B.ring(2, [128, D], F32, "wds")
        wgb = B.ring(2, [128, KD, 128], BF16, "wgb")
        wub = B.ring(2, [128, KD, 128], BF16, "wub")
        wdb = B.ring(2, [128, D], BF16, "wdb")
        sar = B.ring(2, [128, 512], F32, "sa")
        actr = B.ring(2, [128, 512], BF16, "actb")
        ysr = B.ring(3, [128, 512], F32, "ysb")
        psa = Ring([PS[0], PS[1]], "psA")
        psu = Ring([PS[2], PS[3]], "psU")
        psy = Ring([PS[4], PS[5], PS[6], PS[7]], "psY")
        NACT = 2
        NEXP = int(os.environ.get("NEXP", str(NE)))
        items = [(e_, fb) for e_ in range(NEXP) for fb in range(NF)]

        def load_item(i):
            e_, fb = items[i]
            g_s, g_sk = wgs.next(); u_s, u_sk = wus.next(); d_s, d_sk = wds.next()
            S.dma(g_s[:], wgate_d[L, e_, :, fb * 128:(fb + 1) * 128].rearrange("(k p) f -> p k f", p=128), writes=[g_sk])
            S.dma(u_s[:], wup_d[L, e_, :, fb * 128:(fb + 1) * 128].rearrange("(k p) f -> p k f", p=128), writes=[u_sk])
            S.dma(d_s[:], wdown_d[L, e_, fb * 128:(fb + 1) * 128, :], writes=[d_sk])
            g_b, g_bk = wgb.next(); u_b, u_bk = wub.next(); d_b, d_bk = wdb.next()
            B.cp("act", g_b[:], g_s[:], [g_sk], [g_bk])
            B.cp("pool", u_b[:], u_s[:], [u_sk], [u_bk])
            B.cp("act", d_b[:], d_s[:], [d_sk], [d_bk])
            return (g_b, g_bk, u_b, u_bk, d_b, d_bk)

        units = [(i, t) for i in range(len(items)) for t in range(len(tiles))]
        loaded = {0: load_item(0)}
        if len(items) > 1:
            loaded[1] = load_item(1)
        next_to_load = 2
        gwb_of = {}

        def emit_proj(u):
            i, t = units[u]
            e_, fb = items[i]
            c0, cw, isctx = tiles[t]
            if fb == 0 and t == 0:
                gwb, gwbk = gwbr.next()
                for (c0_, cw_, _x) in tiles:
                    pa, pak = psa.next()
                    B.mm(pa[:, :cw_], cm2[:, e_, :], gw[:, c0_:c0_ + cw_], True, True, ["cm2", "gw"], [pak])
                    B.cp("act", gwb[:, c0_:c0_ + cw_], pa[:, :cw_], [pak], [gwbk])
                gwb_of[e_] = (gwb, gwbk)
            gwb, gwbk = gwb_of[e_]
            g_b, g_bk, u_b, u_bk, d_b, d_bk = loaded[i]
            pa, pak = psa.next(); pu, puk = psu.next()
            for k in range(KD):
                B.mm(pa[:, :cw], g_b[:, k, :], h_sb[:, k, c0:c0 + cw], k == 0, k == KD - 1, [g_bk, "h%d" % k], [pak])
            for k in range(KD):
                B.mm(pu[:, :cw], u_b[:, k, :], h_sb[:, k, c0:c0 + cw], k == 0, k == KD - 1, [u_bk, "h%d" % k], [puk])
            sa, sak = sar.next()
            B.act(sa[:, :cw], pa[:, :cw], AF.Silu, [pak], [sak])
            B.tt("dve", sa[:, :cw], sa[:, :cw], pu[:, :cw], ALU.mult, [sak, puk], [sak])
            ab, abk = actr.next()
            B.tt("pool", ab[:, :cw], sa[:, :cw], gwb[:, c0:c0 + cw], ALU.mult, [sak, gwbk], [abk])
            return ab, abk

        def emit_down(u, ab, abk):
            i, t = units[u]
            c0, cw, isctx = tiles[t]
            col = 1 if isctx else 0
            g_b, g_bk, u_b, u_bk, d_b, d_bk = loaded[i]
            for dk in range(KD):
                py, pyk = psy.next()
                B.mm(py[:, :cw], d_b[:, dk * 128:(dk + 1) * 128], ab[:, :cw], True, True, [d_bk, abk], [pyk])
                gsc = modT[:, L, 40 + dk, col:col + 1]
                if dk < NACT:
                    ys, ysk = ysr.next()
                    B.act(ys[:, :cw], py[:, :cw], AF.Identity, [pyk, "modT"], [ysk], scale=gsc)
                    B.tt("pool", x_sb[:, dk, c0:c0 + cw], x_sb[:, dk, c0:c0 + cw], ys[:, :cw], ALU.add,
                         ["x%d" % dk, ysk], ["x%d" % dk])
                else:
                    B.stt("dve", x_sb[:, dk, c0:c0 + cw], py[:, :cw], gsc, x_sb[:, dk, c0:c0 + cw],
                          ALU.mult, ALU.add, [pyk, "modT", "x%d" % dk], ["x%d" % dk])

        cur = emit_proj(0)
        for u in range(len(units)):
            nxt = emit_proj(u + 1) if u + 1 < len(units) else None
            emit_down(u, cur[0], cur[1])
            i, t = units[u]
            if t == len(tiles) - 1:
                del loaded[i]
                if next_to_load < len(items):
                    loaded[next_to_load] = load_item(next_to_load)
                    next_to_load += 1
            cur = nxt
        B.release(mk)

    moe_layer(0, True)
    if stage == 3:
        return finish("x")

    LAM_INIT = 0.8 - 0.6 * math.exp(-0.3 * 1)
    norm_mod(2, True)
    ma = B.mark()
    q_all = B.sb([128, 8, NL], BF16, "qall")
    kctx = B.sb([128, 8, NCX], BF16, "kctx")
    vctx = B.sb([128, 8, 2, 128], BF16, "vctx")
    nlam = B.sb([128, 4], F32, "nlam")
    mlam = B.mark()
    lvb = B.sb([128, 256], F32, "lvb")
    S.dma(lvb[:], lvb_d, writes=["lvb"])
    lvp = B.sb([128, 128], F32, "lvp")
    B.tt("dve", lvp[:, 0:64], lvb[:, 0:64], lvb[:, 64:128], ALU.mult, ["lvb"], ["lvp"])
    B.tt("dve", lvp[:, 64:128], lvb[:, 128:192], lvb[:, 192:256], ALU.mult, ["lvb"], ["lvp"])
    S.op("dve", lambda e: e.tensor_reduce(out=nlam[:, 2:3], in_=lvp[:, 0:64], axis=AX.X, op=ALU.add), reads=["lvp"], writes=["nlam"])
    S.op("dve", lambda e: e.tensor_reduce(out=nlam[:, 3:4], in_=lvp[:, 64:128], axis=AX.X, op=ALU.add), reads=["lvp"], writes=["nlam"])
    B.act(nlam[:, 2:4], nlam[:, 2:4], AF.Exp, ["nlam"], ["nlam"])
    B.tt("dve", nlam[:, 0:1], nlam[:, 3:4], nlam[:, 2:3], ALU.subtract, ["nlam"], ["nlam"])
    B.ts("dve", nlam[:, 0:1], nlam[:, 0:1], -LAM_INIT, None, ALU.add, None, ["nlam"], ["nlam"])
    B.ts("dve", nlam[:, 1:2], vecs[:, 57:58], 1.0 - LAM_INIT, None, ALU.mult, None, ["vecs"], ["nlam"])
    B.release(mlam)

    cin_k = [B.dram_tmp("cin_k%d" % i, [128, NL], BF16) for i in range(8)]
    cout_k = [B.dram_tmp("cout_k%d" % i, [512, NL], BF16) for i in range(8)]
    cin_v = [B.dram_tmp("cin_v%d" % i, [NL, 128], BF16) for i in range(8)]
    cout_v = [B.dram_tmp("cout_v%d" % i, [4 * NL, 128], BF16) for i in range(8)]

    mpa = B.mark()
    rope = B.sb([128, 2, NL], F32, "rope")
    S.dma(rope[:], rope_d, writes=["rope"])
    wst2 = B.ring(2, [128, KD, 128], F32, "wst2")
    wbf2 = B.ring(3, [128, KD, 128], BF16, "wbf2")
    rtmp = B.ring(4, [128, 512], F32, "rtmp")
    kT_loc = B.ring(1, [128, NL], BF16, "kTloc")
    v_loc = B.ring(1, [128, NL // 128, 128], BF16, "vloc")

    def load_w2(j):
        st_, stk = wst2.next()
        S.dma(st_[:], dw_in_d[:, j * 128:(j + 1) * 128].rearrange("(k p) c -> p k c", p=128), writes=[stk])
        wb, wbk = wbf2.next()
        B.cp("pool", wb[:], st_[:], [stk], [wbk])
        return wb, wbk

    def rope_tile(ps, psk, c0, out_ap, outk):
        xf, xfk = rtmp.next()
        B.cp("act", xf[:], ps[:], [psk], [xfk])
        B.mm(PS[2][:], cmat[:, 2, :], xf[:], True, True, ["cmat", xfk], ["ps2"])
        t1, t1k = rtmp.next()
        B.tt("pool", t1[:], xf[:], rope[:, 0, c0:c0 + 512], ALU.mult, [xfk, "rope"], [t1k])
        t2, t2k = rtmp.next()
        B.tt("dve", t2[:], PS[2][:], rope[:, 1, c0:c0 + 512], ALU.mult, ["ps2", "rope"], [t2k])
        B.tt("dve", out_ap, t1[:], t2[:], ALU.add, [t1k, t2k], [outk])

    for hd in range(8):
        wq, wqk = load_w2(hd)
        wk_, wkk = load_w2(8 + hd)
        wv, wvk = load_w2(16 + hd)
        kT, kTk = kT_loc.next()
        for (c0, cw, isctx) in col_tiles(True):
            if not isctx:
                proj_fm(PS[0], "ps0", wq, wqk, c0, cw)
                rope_tile(PS[0], "ps0", c0, q_all[:, hd, c0:c0 + cw], "qall%d" % hd)
                proj_fm(PS[1], "ps1", wk_, wkk, c0, cw)
                rope_tile(PS[1], "ps1", c0, kT[:, c0:c0 + cw], kTk)
            else:
                proj_fm(PS[1], "ps1", wk_, wkk, c0, cw)
                B.cp("act", kctx[:, hd, :], PS[1][:, :cw], ["ps1"], ["kctx"])
        S.dma(cin_k[hd], kT[:], reads=[kTk], writes=["cin_k%d" % hd])
        S.coll(lambda e, hd=hd: e.collective_compute("AllGather", ALU.bypass, replica_groups=[[0, 1, 2, 3], [4, 5, 6, 7]],
                                                      ins=[cin_k[hd]], outs=[cout_k[hd]]),
             reads=["cin_k%d" % hd], writes=["cout_k%d" % hd])
        vl, vlk = v_loc.next()
        for tj in range(NT // 128):
            bank_col = (tj % 4) * 128
            for k in range(KD):
                B.mm(PS[3][:, bank_col:bank_col + 128], h_sb[:, k, tj * 128:(tj + 1) * 128], wv[:, k, :],
                     k == 0, k == KD - 1, ["h%d" % k, wvk], ["ps3"])
            if tj % 4 == 3 and tj < 16:
                B.cp("act", vl[:, tj - 3:tj + 1, :], PS[3][:].rearrange("p (j v) -> p j v", v=128), ["ps3"], [vlk])
            if tj == 17:
                B.cp("act", vctx[:, hd, :, :], PS[3][:, 0:256].rearrange("p (j v) -> p j v", v=128), ["ps3"], ["vctx"])
        S.dma(cin_v[hd].rearrange("(j p) v -> p j v", p=128), vl[:], reads=[vlk], writes=["cin_v%d" % hd])
        S.coll(lambda e, hd=hd: e.collective_compute("AllGather", ALU.bypass, replica_groups=[[0, 1, 2, 3], [4, 5, 6, 7]],
                                                      ins=[cin_v[hd]], outs=[cout_v[hd]]),
             reads=["cin_v%d" % hd], writes=["cout_v%d" % hd])
    cin_zk = B.dram_tmp("cin_zk", [16, 64])
    cout_zk = B.dram_tmp("cout_zk", [64, 64])
    allkv = ["cout_k%d" % i for i in range(8)] + ["cout_v%d" % i for i in range(8)]
    S.dma(cin_zk, vecs[0:16, 0:64], reads=["vecs"] + allkv, writes=["cin_zk"])
    S.coll(lambda e: e.collective_compute("AllGather", ALU.bypass, replica_groups=[[0, 1, 2, 3], [4, 5, 6, 7]],
                                          ins=[cin_zk], outs=[cout_zk]), reads=["cin_zk"] + allkv, writes=["cout_zk"] + allkv)
    B.release(mpa)
    if stage == 35:
        return finish("x")

    NKT = (4 * NL + NCX) // 128
    h_off = B.SB_BASE + 128 * 0 + KD * NT * 4
    K01 = nc.alloc_sbuf_tensor_at("K01", [128, 2, NKT * 128], BF16, offset=h_off)
    B.memset("pool", K01[64:128, 0, :], 0.0, ["K01"])
    B.memset("pool", K01[0:64, 1, :], 0.0, ["K01"])
    V_h = B.sb([128, NKT, 128], BF16, "Vh")
    er = B.ring(4, [128, 512], BF16, "etl")
    ftmp = B.ring(6, [128, 512], F32, "ftmp")
    onr2 = B.ring(2, [128, 512], BF16, "on2")
    wo_st2 = B.sb([128, D], F32, "wost2")
    wo_bf2 = B.sb([128, D], BF16, "wobf2")
    psS = [Ring([PS[0], PS[1]], "psS0"), Ring([PS[2], PS[3]], "psS1")]
    NKT_RUN = int(os.environ.get("NKT", str(NKT)))
    for hd in range(8):
        for r_ in range(4):
            for c_ in range(2):
                S.dma(K01[c_ * 64:(c_ + 1) * 64, c_, r_ * NL:(r_ + 1) * NL],
                      cout_k[hd][r_ * 128 + c_ * 64:r_ * 128 + (c_ + 1) * 64, :], reads=["cout_k%d" % hd, "K01"], writes=["K01"])
            S.dma(V_h[:, r_ * 16:(r_ + 1) * 16, :], cout_v[hd][r_ * NL:(r_ + 1) * NL, :].rearrange("(j p) v -> p j v", p=128),
                  reads=["cout_v%d" % hd, "Vh"], writes=["Vh"])
        for c_ in range(2):
            B.cp("pool", K01[c_ * 64:(c_ + 1) * 64, c_, 4 * NL:4 * NL + NCX], kctx[c_ * 64:(c_ + 1) * 64, hd, :], ["kctx", "K01"], ["K01"])
        B.cp("pool", V_h[:, 64:66, :], vctx[:, hd, :, :], ["vctx", "Vh"], ["Vh"])
        S.dma(wo_st2[:], dw_out_d[hd * 128:(hd + 1) * 128, :], writes=["wost2"])
        B.cp("pool", wo_bf2[:], wo_st2[:], ["wost2"], ["wobf2"])
        for qt in range(NL // 512):
            qc = slice(qt * 512, (qt + 1) * 512)
            for kt in range(NKT_RUN):
                kc = slice(kt * 128, (kt + 1) * 128)
                first, last = kt == 0, kt == NKT_RUN - 1
                for c_ in range(2):
                    ps_, psk = psS[c_].next()
                    B.mm(ps_[:], K01[:, c_, kc], q_all[:, hd, qc], True, True, ["K01", "qall%d" % hd], [psk])
                    et, etk = er.next()
                    B.act(et[:], ps_[:], AF.Exp, [psk], [etk], scale=DIFF_SCALE)
                    B.mm(PS[4 + c_][:], V_h[:, kt, :], et[:], first, last, ["Vh", etk], ["ps%d" % (4 + c_)])
                    B.mm(PS[6 + c_][:], ones_b[:], et[:], first, last, ["onesb", etk], ["ps%d" % (6 + c_)])
            oc = []
            for c_ in range(2):
                rc, rck = ftmp.next()
                S.op("dve", lambda e, o=rc[:], i_=PS[6 + c_][:]: e.reciprocal(out=o, in_=i_), reads=["ps%d" % (6 + c_)], writes=[rck])
                B.tt("dve", rc[:], PS[4 + c_][:], rc[:], ALU.mult, ["ps%d" % (4 + c_), rck], [rck])
                oc.append((rc, rck))
            o_, ok_ = ftmp.next()
            B.stt("dve", o_[:], oc[1][0][:], nlam[:, 0:1], oc[0][0][:], ALU.mult, ALU.add, [oc[1][1], oc[0][1], "nlam"], [ok_])
            sq, sqk = er.next()
            B.act(sq[:], o_[:], AF.Square, [ok_], [sqk])
            ps_, psk = psS[0].next()
            B.mm(ps_[:], m128_b[:], sq[:], True, True, [sqk, "m128b"], [psk])
            rs, rsk = ftmp.next()
            B.rsqrt(rs[:], ps_[:], [psk], rsk)
            B.tt("dve", o_[:], o_[:], rs[:], ALU.mult, [ok_, rsk], [ok_])
            on, onk = onr2.next()
            B.ts("dve", on[:], o_[:], nlam[:, 1:2], None, ALU.mult, None, [ok_, "nlam"], [onk])
            for dk in range(KD):
                ps_, psk = psS[dk % 2].next()
                B.mm(ps_[:], wo_bf2[:, dk * 128:(dk + 1) * 128], on[:], True, True, ["wobf2", onk], [psk])
                B.stt("dve", x_sb[:, dk, qc], ps_[:], modT[:, 1, 16 + dk, 0:1], x_sb[:, dk, qc], ALU.mult, ALU.add,
                      [psk, "modT", "x%d" % dk], ["x%d" % dk])
    B.release(ma)
    if stage == 4:
        return finish("x")

    moe_layer(1, False)

    mf = B.mark()
    sqr = B.ring(3, [128, 512], BF16, "fsq")
    rsr = B.ring(2, [128, 512], F32, "frs")
    otr = B.ring(3, [128, 512], F32, "fout")
    for (c0, cw, isctx) in col_tiles(False):
        for k in range(KD):
            sq, sqk = sqr.next()
            B.act(sq[:], x_sb[:, k, c0:c0 + cw], AF.Square, ["x%d" % k], [sqk])
            B.mm(PS[1][:], mean_b[:], sq[:], k == 0, k == KD - 1, [sqk, "meanb"], ["ps1"])
        rs, rsk = rsr.next()
        B.rsqrt(rs[:], PS[1][:], ["ps1"], rsk)
        for k in range(KD):
            ot, otk = otr.next()
            B.stt("dve", ot[:], x_sb[:, k, c0:c0 + cw], vecs[:, 32 + k:33 + k], rs[:], ALU.mult, ALU.mult,
                  ["x%d" % k, "vecs", rsk], [otk])
            S.dma(out_d[k * 128:(k + 1) * 128, c0:c0 + cw], ot[:], reads=[otk], writes=["out%d" % k])
    S.wait_all("sp", ["out%d" % k for k in range(KD)])
    if dbg:
        for k in range(KD):
            S.dma(dbg_d[k * 128:(k + 1) * 128, :], x_sb[:, k, :], reads=["x%d" % k], writes=["dbg%d" % k])
        S.wait_all("sp", ["dbg%d" % k for k in range(KD)])
    S.emit()
    return nc

    return finish("x")


def _consts():
    ident = np.eye(128, dtype=np.float32)
    ones = np.ones((128, 128), np.float32)
    PT = np.zeros((128, 128), np.float32)
    for p in range(128):
        j = p % 64
        if j < 32:
            PT[p + 32, p] = -1.0
        else:
            PT[p - 32, p] = 1.0
    cmat = np.stack([ident, ones, PT, np.zeros((128, 128), np.float32)], axis=1)
    s = np.arange(128)[:, None]
    t = np.arange(128)[None, :]
    same = (s // 64) == (t // 64)
    mfw = (same & (s <= t)).astype(np.int32)
    mbw = (same & (s >= t)).astype(np.int32)
    masks = np.stack([mfw, mbw], axis=1)
    scanm = np.ones((128, 512), np.float32)
    scanm[:, ::64] = 0.0
    cm2 = np.zeros((128, 18, 128), np.float32)
    for e in range(16):
        cm2[e, e, :] = 1.0
    cm2[0:16, 16, :] = 1.0
    for p in range(80):
        for q in range(80):
            if (p < 64 and q < 64 and p % 16 == q % 16) or (p >= 64 and q == p):
                cm2[p, 17, q] = 1.0
    _consts.cm2 = cm2
    return np.ascontiguousarray(cmat), np.ascontiguousarray(masks), scanm


def make_in_maps(inputs):
    f = lambda a: np.ascontiguousarray(np.asarray(a, dtype=np.float32))
    x, c, ctx, c_ctx = f(inputs["x"]), f(inputs["c"]), f(inputs["ctx"]), f(inputs["c_ctx"])
    cmat, masks, scanm = _consts()
    vecs = np.zeros((128, 64), np.float32)
    pk = lambda v: np.asarray(v, np.float32).reshape(8, 128).T
    nm, nf = f(inputs["norm_mix"]), f(inputs["norm_ffn"])
    vecs[:, 0:8] = pk(nm[0]); vecs[:, 8:16] = pk(nm[1])
    vecs[:, 16:24] = pk(nf[0]); vecs[:, 24:32] = pk(nf[1])
    vecs[:, 32:40] = pk(f(inputs["norm_final"]))
    lbl = f(inputs["hgrn_lb_logits"])
    vecs[:, 40:48] = pk(lbl[0]); vecs[:, 48:56] = pk(lbl[1])
    vecs[:, 56] = f(inputs["hgrn_norm"])[0]
    vecs[:, 57] = f(inputs["diff_subln"])[0]
    vecs[0:64, 58] = float(CAP_L)
    vecs[64:128, 58] = float(CAP_C)
    adab = np.ascontiguousarray(f(inputs["ada_b"]).reshape(2, 48, 128).transpose(2, 0, 1))
    shared = {
        "ada_w": f(inputs["ada_w"]), "adab": adab, "vecs": vecs, "cmat": cmat, "masks": masks, "scanm": scanm,
        "hgrn_w_in": f(inputs["hgrn_w_in"])[0], "hgrn_w_out": f(inputs["hgrn_w_out"])[0],
        "diff_w_in": f(inputs["diff_w_in"])[0], "diff_w_out": f(inputs["diff_w_out"])[0],
        "lvb": np.ascontiguousarray(np.tile(f(inputs["diff_lambda"])[0].reshape(1, 256), (128, 1))),
        "cm2": _consts.cm2, "moe_router": f(inputs["moe_router"]), "moe_w_gate": f(inputs["moe_w_gate"]),
        "moe_w_up": f(inputs["moe_w_up"]), "moe_w_down": f(inputs["moe_w_down"]),
    }
    in_maps = []
    for core in range(NCORES):
        b, seg = core // 4, core % 4
        xT = np.ascontiguousarray(np.concatenate([x[b, seg * NL:(seg + 1) * NL, :], ctx[b]], axis=0).T)
        cT = np.ascontiguousarray(np.stack([c[b], c_ctx], axis=1))
        segm = np.zeros((128, 16), np.float32)
        for s in range(4):
            segm[:, s] = 1.0 if s < seg else 0.0
            segm[:, 4 + s] = 1.0 if s > seg else 0.0
        segm[:, 8:16] = 1.0 - segm[:, 0:8]
        tg = seg * NL + np.arange(NL)
        freq = (10000.0 ** (-np.arange(16, dtype=np.float32) / 16.0)).astype(np.float32)
        ang = np.concatenate([(tg // 64).astype(np.float32)[:, None] * freq, (tg % 64).astype(np.float32)[:, None] * freq], axis=-1)
        pj = np.arange(128) % 32
        rope = np.ascontiguousarray(np.stack([np.cos(ang)[:, pj].T, np.sin(ang)[:, pj].T], axis=1).astype(np.float32))
        m = dict(shared)
        m.update({"xT": xT, "cT": cT, "segm": segm, "rope": rope})
        in_maps.append(m)
    return in_maps


def run(inputs, stage=99, dbg=False):
    nc = build(stage, dbg)
    in_maps = make_in_maps(inputs)
    import os
    n1 = int(os.environ.get("NC1", "0"))
    if n1:
        return run_bass_kernel_spmd(nc, in_maps[:n1], core_ids=list(range(n1)))
    res = run_bass_kernel_spmd(nc, in_maps, core_ids=list(range(NCORES)))
    return res


def kernel(**inputs):
    res = run(inputs, stage=99)
    out = np.zeros((2, 8192, D), np.float32)
    for core in range(NCORES):
        b, seg = core // 4, core % 4
        out[b, seg * NL:(seg + 1) * NL, :] = res.results[core]["outT"].T
    return out
```
